# Optimizing a Trainium2 kernel written in Bass

```python
import jax, jax.numpy as jnp
from jax import lax
import numpy as np

D_MODEL = 1024
BATCH = 16
SEQ = 2048
DEPTH = 2

CHUNK = 64
N_META = 16
GLA_HEADS = 4
GLA_KEY = D_MODEL // 2
GLA_VAL = D_MODEL
GLA_DK = GLA_KEY // GLA_HEADS
GLA_DV = GLA_VAL // GLA_HEADS
GATE_RANK = 16
GATE_NORMALIZER = 16.0
POOL_WINDOWS = (2, 4, 8, 16)
POOL_GROUPS = 4
POOL_WIDTH = D_MODEL
POOL_GDIM = POOL_WIDTH // POOL_GROUPS
N_BRANCH = 2
IN_COLS = 2 * GLA_KEY + 2 * GLA_VAL + GATE_RANK + POOL_WIDTH + N_BRANCH * D_MODEL
N_GROUPS = 4
EXPERTS_PER_GROUP = 4
N_EXPERTS = N_GROUPS * EXPERTS_PER_GROUP
TOP_K = 2
D_EXPERT = D_MODEL // 2
EPS = 1e-6

kernel_name = "chunk_causal_gla_pool_hmoe_hybrid"


def rms_norm(x, w):
    xf = x.astype(jnp.float32)
    y = xf * lax.rsqrt(jnp.mean(xf * xf, axis=-1, keepdims=True) + EPS)
    return (y * w.astype(jnp.float32)).astype(x.dtype)


def gla_mixer(q, k, v, og, g_low, w_gate_up, b_gate, norm_w):
    B, L, _ = q.shape
    dt = q.dtype
    pad = (-L) % CHUNK
    n_chunks = (L + pad) // CHUNK
    g = jax.nn.log_sigmoid((g_low @ w_gate_up + b_gate).astype(jnp.float32)) / GATE_NORMALIZER

    def to_chunks(t, hd):
        t = jnp.pad(t, ((0, 0), (pad, 0), (0, 0)))
        return t.reshape(B, n_chunks, CHUNK, GLA_HEADS, hd)

    qc = to_chunks(q * (GLA_DK ** -0.5), GLA_DK)
    kc = to_chunks(k, GLA_DK)
    vc = to_chunks(v, GLA_DV)
    gc = to_chunks(g, GLA_DK)
    b = jnp.cumsum(gc, axis=2)
    gam = b[:, :, -1]
    eb = jnp.exp(b).astype(dt)
    inv_eb = jnp.exp(-b).astype(dt)
    q_eb = qc * eb
    a_lo = jnp.einsum('bnihk,bnjhk->bnhij', q_eb, kc * inv_eb)
    a_up = jnp.einsum('bnihk,bnjhk->bnhij', qc * inv_eb, kc * eb)
    pos = jnp.arange(CHUNK)
    attn = jnp.where(pos[:, None] >= pos[None, :], a_lo, a_up)
    o_intra = jnp.einsum('bnhij,bnjhv->bnihv', attn, vc)
    k_dec = kc * jnp.exp(gam[:, :, None] - b).astype(dt)
    dec = jnp.exp(gam).astype(dt)

    def step(state, inp):
        q_t, k_t, v_t, d_t = inp
        o_t = jnp.einsum('bihk,bhkv->bihv', q_t, state)
        state = d_t[..., None] * state + jnp.einsum('bjhk,bjhv->bhkv', k_t, v_t)
        return state, o_t

    xs = (jnp.moveaxis(q_eb, 1, 0), jnp.moveaxis(k_dec, 1, 0),
          jnp.moveaxis(vc, 1, 0), jnp.moveaxis(dec, 1, 0))
    s0 = jnp.zeros((B, GLA_HEADS, GLA_DK, GLA_DV), dt)
    _, o_inter = lax.scan(step, s0, xs)
    o = o_intra + jnp.moveaxis(o_inter, 0, 1)
    o = rms_norm(o, norm_w)
    o = o.reshape(B, n_chunks * CHUNK, GLA_VAL)[:, pad:]
    return o * jax.nn.silu(og)


def pool_mixer(u, w_grp, scale):
    B, L, _ = u.shape
    uf = u.astype(jnp.float32)
    cs = jnp.concatenate([jnp.zeros((B, 1, POOL_WIDTH), jnp.float32),
                          jnp.cumsum(uf, axis=1)], axis=1)
    t = jnp.arange(L)
    outs = []
    for gi, w in enumerate(POOL_WINDOWS):
        sl = slice(gi * POOL_GDIM, (gi + 1) * POOL_GDIM)
        c = cs[:, :, sl]
        upper = c[:, 1:]
        lower = jnp.pad(c, ((0, 0), (w - 1, 0), (0, 0)))[:, :L]
        count = jnp.minimum(t + 1, w).astype(jnp.float32)[None, :, None]
        outs.append((upper - lower) / count - uf[:, :, sl])
    pooled = jnp.stack(outs, axis=2).astype(u.dtype)
    mixed = jnp.einsum('blgc,gcd->blgd', pooled, w_grp).reshape(B, L, POOL_WIDTH)
    return mixed * scale


def hier_moe(h, w_rg, w_re, w_eg, w_eu, w_ed):
    B, L, D = h.shape
    xt = h.reshape(-1, D)
    T = xt.shape[0]
    pg = jax.nn.softmax((xt @ w_rg).astype(jnp.float32), axis=-1)
    pg_top, g_idx = lax.top_k(pg, 1)
    le = (xt @ w_re).astype(jnp.float32).reshape(T, N_GROUPS, EXPERTS_PER_GROUP)
    le_sel = jnp.take_along_axis(le, g_idx[:, :, None], axis=1)[:, 0]
    pe = jax.nn.softmax(le_sel, axis=-1)
    pe_top, e_idx = lax.top_k(pe, TOP_K)
    wts = pg_top * pe_top / jnp.sum(pe_top, axis=-1, keepdims=True)
    gid = g_idx * EXPERTS_PER_GROUP + e_idx
    comb = jnp.einsum('tk,tke->te', wts,
                      jax.nn.one_hot(gid, N_EXPERTS, dtype=jnp.float32)).astype(h.dtype)
    out = jnp.zeros_like(xt)
    for e in range(N_EXPERTS):
        hid = jax.nn.silu(xt @ w_eg[e]) * (xt @ w_eu[e])
        out = out + comb[:, e:e + 1] * (hid @ w_ed[e])
    return out.reshape(B, L, D)


def setup_inputs(seed: int = 0) -> dict:
    key = jax.random.key(seed)
    ks = jax.random.split(key, 20)
    f32 = jnp.float32

    def nrm(k, shape, scale):
        return jax.random.normal(k, shape, f32) * scale

    return {
        "x": nrm(ks[0], (BATCH, SEQ, D_MODEL), 1.0),
        "meta_tokens": nrm(ks[1], (N_META, D_MODEL), 1.0),
        "norm1_w": 1.0 + nrm(ks[2], (DEPTH, D_MODEL), 0.05),
        "w_in": nrm(ks[3], (DEPTH, D_MODEL, IN_COLS), D_MODEL ** -0.5),
        "w_gate_up": nrm(ks[4], (DEPTH, GATE_RANK, GLA_KEY), GATE_RANK ** -0.5),
        "b_gate": nrm(ks[5], (DEPTH, GLA_KEY), 0.1),
        "gla_norm_w": 1.0 + nrm(ks[6], (DEPTH, GLA_DV), 0.05),
        "w_pool_grp": nrm(ks[7], (DEPTH, POOL_GROUPS, POOL_GDIM, POOL_GDIM), POOL_GDIM ** -0.5),
        "pool_scale": 1.0 + nrm(ks[8], (DEPTH, POOL_WIDTH), 0.1),
        "w_br_gla": nrm(ks[9], (DEPTH, GLA_VAL, D_MODEL), GLA_VAL ** -0.5),
        "w_br_pool": nrm(ks[10], (DEPTH, POOL_WIDTH, D_MODEL), POOL_WIDTH ** -0.5),
        "w_out": nrm(ks[11], (DEPTH, D_MODEL, D_MODEL), D_MODEL ** -0.5),
        "norm2_w": 1.0 + nrm(ks[12], (DEPTH, D_MODEL), 0.05),
        "w_router_group": nrm(ks[13], (DEPTH, D_MODEL, N_GROUPS), D_MODEL ** -0.5),
        "w_router_expert": nrm(ks[14], (DEPTH, D_MODEL, N_EXPERTS), D_MODEL ** -0.5),
        "w_exp_gate": nrm(ks[15], (DEPTH, N_EXPERTS, D_MODEL, D_EXPERT), D_MODEL ** -0.5),
        "w_exp_up": nrm(ks[16], (DEPTH, N_EXPERTS, D_MODEL, D_EXPERT), D_MODEL ** -0.5),
        "w_exp_down": nrm(ks[17], (DEPTH, N_EXPERTS, D_EXPERT, D_MODEL), D_EXPERT ** -0.5),
        "final_norm_w": 1.0 + nrm(ks[18], (D_MODEL,), 0.05),
    }


def reference(x, meta_tokens, norm1_w, w_in, w_gate_up, b_gate, gla_norm_w, w_pool_grp,
              pool_scale, w_br_gla, w_br_pool, w_out, norm2_w, w_router_group,
              w_router_expert, w_exp_gate, w_exp_up, w_exp_down, final_norm_w):
    B = x.shape[0]
    meta = jnp.broadcast_to(meta_tokens[None].astype(x.dtype), (B, N_META, D_MODEL))
    h = jnp.concatenate([meta, x], axis=1)
    splits = [GLA_KEY, 2 * GLA_KEY, 2 * GLA_KEY + GLA_VAL, 2 * GLA_KEY + 2 * GLA_VAL,
              2 * GLA_KEY + 2 * GLA_VAL + GATE_RANK,
              2 * GLA_KEY + 2 * GLA_VAL + GATE_RANK + POOL_WIDTH]
    for l in range(DEPTH):
        hn = rms_norm(h, norm1_w[l])
        z = hn @ w_in[l]
        q, k, v, og, g_low, u, gate_cols = jnp.split(z, splits, axis=-1)
        y_gla = gla_mixer(q, k, v, og, g_low, w_gate_up[l], b_gate[l], gla_norm_w[l])
        y_pool = pool_mixer(u, w_pool_grp[l], pool_scale[l])
        gates = jax.nn.sigmoid(gate_cols.reshape(B, -1, N_BRANCH, D_MODEL))
        merged = (gates[:, :, 0] * (y_gla @ w_br_gla[l])
                  + gates[:, :, 1] * (y_pool @ w_br_pool[l]))
        h = h + merged @ w_out[l]
        h = h + hier_moe(rms_norm(h, norm2_w[l]), w_router_group[l], w_router_expert[l],
                         w_exp_gate[l], w_exp_up[l], w_exp_down[l])
    h = rms_norm(h, final_norm_w)
    return h[:, N_META:]
```

```python
import contextlib
import numpy as np
import concourse.bass as bass
import concourse.mybir as mybir
from concourse.bass_utils import run_bass_kernel_spmd

F32 = mybir.dt.float32
BF16 = mybir.dt.bfloat16
U8 = mybir.dt.uint8
AF = mybir.ActivationFunctionType
ALU = mybir.AluOpType

ENGS = ["pe", "act", "dve", "pool", "sp"]

D = 1024
NL = 2
SEQ = 2048
NMETA = 16
NT_SEQ = 17
GROUPS = [(0, 9), (9, 8)]
TMAX = 9 * 128
IN_COLS = 6160
C_Q, C_K, C_V, C_OG, C_GL, C_U, C_G0, C_G1 = 0, 512, 1024, 2048, 3072, 3088, 4112, 5136
EPS = 1e-6
QSCALE = 128.0 ** -0.5
NEXP = 16

PV_N1, PV_N2, PV_PS, PV_BG, PV_GN = 0, 8, 16, 24, 28
PV_L = 30
PV_FN = 2 * PV_L
PV_COLS = PV_FN + 8


class Op:
    __slots__ = ("eng", "fn", "deps", "dmadeps", "idx", "group", "milestone", "count", "name")


class Prog:
    def __init__(self, nc):
        self.nc = nc
        self.ops = {e: [] for e in ENGS}
        self.lastw = {}
        self.readers = {}
        self.dma_total = {}
        self.stack = contextlib.ExitStack()

    def sb(self, name, shape, dt):
        return self.stack.enter_context(self.nc.sbuf_tensor(name, list(shape), dt))

    def ps(self, name, shape, dt=F32):
        return self.stack.enter_context(self.nc.psum_tensor(name, list(shape), dt))

    def add(self, eng, fn, reads=(), writes=(), group=None, name=""):
        op = Op()
        op.eng = eng
        op.fn = fn
        op.group = group
        op.name = name or getattr(self, 'phase', '')
        op.milestone = False
        op.count = 0
        op.idx = len(self.ops[eng])
        deps = set()
        dmadeps = {}

        def dep_on(o):
            if o.group is not None:
                g = o.group
                dmadeps[g] = max(dmadeps.get(g, 0), self.dma_total[g])
            else:
                deps.add(o)

        for k in reads:
            o = self.lastw.get(k)
            if o is not None:
                dep_on(o)
        for k in writes:
            o = self.lastw.get(k)
            if o is not None:
                dep_on(o)
            for r in self.readers.get(k, ()):
                if r.eng == eng and eng == "pe" and group is None:
                    continue
                dep_on(r)
        if group is not None:
            self.dma_total[group] = self.dma_total.get(group, 0) + 1
        fdeps = set()
        for o in deps:
            if o.eng == "pe" and eng == "pe" and group is None:
                continue
            fdeps.add(o)
        op.deps = fdeps
        op.dmadeps = dmadeps
        for k in reads:
            self.readers.setdefault(k, []).append(op)
        for k in writes:
            self.lastw[k] = op
            self.readers[k] = []
        self.ops[eng].append(op)
        return op

    def emit(self):
        nc = self.nc
        for e in ENGS:
            for op in self.ops[e]:
                for d in op.deps:
                    d.milestone = True
        for e in ENGS:
            c = 0
            for op in self.ops[e]:
                if op.group is None and op.milestone:
                    c += 1
                    op.count = c
        st = self.stack
        sems = {e: st.enter_context(nc.semaphore("s_" + e)) for e in ENGS}
        gsems = {g: st.enter_context(nc.semaphore("g_%s" % (g,))) for g in self.dma_total}
        block = st.enter_context(nc.Block())
        ops = self.ops

        def run(e, engobj):
            waited = {}
            for op in ops[e]:
                need = {}
                for d in op.deps:
                    key = ("e", d.eng)
                    need[key] = max(need.get(key, 0), d.count)
                for g, tot in op.dmadeps.items():
                    need[("g", g)] = max(need.get(("g", g), 0), tot * 16)
                for key, val in need.items():
                    if waited.get(key, 0) >= val:
                        continue
                    waited[key] = val
                    sem = sems[key[1]] if key[0] == "e" else gsems[key[1]]
                    engobj.wait_ge(sem, val)
                inst = op.fn(engobj)
                op.fn = inst
                if op.group is not None:
                    inst.then_inc(gsems[op.group], 16)
                elif op.milestone:
                    inst.then_inc(sems[e], 1)
            if e == "sp":
                for g, tot in self.dma_total.items():
                    engobj.wait_ge(gsems[g], tot * 16)

        @block.tensor
        def _(eng):
            run("pe", eng)

        @block.scalar
        def _(eng):
            run("act", eng)

        @block.vector
        def _(eng):
            run("dve", eng)

        @block.gpsimd
        def _(eng):
            run("pool", eng)

        @block.sync
        def _(eng):
            run("sp", eng)

    def close(self):
        self.stack.close()


def MM(out, lhsT, rhs, start, stop):
    return lambda e: e.matmul(out, lhsT=lhsT, rhs=rhs, start=start, stop=stop)


def TR(out, in_, identity):
    return lambda e: e.transpose(out=out, in_=in_, identity=identity)


def ACT(out, in_, func, **kw):
    return lambda e: e.activation(out=out, in_=in_, func=func, **kw)


def TT(out, in0, in1, op):
    return lambda e: e.tensor_tensor(out=out, in0=in0, in1=in1, op=op)


def STT(out, in0, scalar, in1, op0, op1):
    return lambda e: e.scalar_tensor_tensor(out=out, in0=in0, scalar=scalar, in1=in1, op0=op0, op1=op1)


def TS(out, in0, scalar1, op0):
    return lambda e: e.tensor_scalar(out=out, in0=in0, scalar1=scalar1, scalar2=None, op0=op0)


def TRED(out, in_, op):
    return lambda e: e.tensor_reduce(out=out, in_=in_, axis=mybir.AxisListType.X, op=op)


def CPY(out, in_):
    return lambda e: e.tensor_copy(out=out, in_=in_)


def MSET(ap, v):
    return lambda e: e.memset(ap, v)


def DMA(out, in_):
    return lambda e: e.dma_start(out=out, in_=in_)


def SCAN(out, data0, data1):
    return lambda e: e.tensor_tensor_scan(out=out, data0=data0, data1=data1, initial=0.0, op0=ALU.mult, op1=ALU.add)


def CPRED(out, mask, data):
    return lambda e: e.copy_predicated(out=out, mask=mask, data=data)


def RECIP(out, in_):
    return lambda e: e.reciprocal(out=out, in_=in_)


def I(name, *args, **kw):
    return lambda e: getattr(e, name)(*args, **kw)


def ntiles_of(ntl):
    out = []
    t = 0
    while t < ntl:
        n = min(4, ntl - t)
        out.append((t, n))
        t += n
    return out


def build_program(debug=None, n_seq=2, n_layers=NL, groups=GROUPS, phases=('n1', 'gla', 'br0', 'pool', 'br1', 'n2', 'route', 'moe')):
    nc = bass.Bass("TRN2", target_bir_lowering=False)

    def din(name, shape, dt=F32):
        return nc.dram_tensor(name, list(shape), dt, kind="ExternalInput").ap()

    x = din("x", [2, SEQ, D])
    meta = din("meta", [NMETA, D])
    w_in = din("w_in", [NL, D, IN_COLS])
    w_gu = din("w_gate_up", [NL, 16, 512])
    w_pool = din("w_pool_grp", [NL, 4, 256, 256])
    w_brg = din("w_br_gla", [NL, D, D])
    w_brp = din("w_br_pool", [NL, D, D])
    w_out = din("w_out", [NL, D, D])
    w_rt = din("w_router", [NL, D, 20])
    w_eg = din("w_exp_gate", [NL, NEXP, D, 512])
    w_eu = din("w_exp_up", [NL, NEXP, D, 512])
    w_ed = din("w_exp_down", [NL, NEXP, 512, D])
    pvec_d = din("pvec", [128, PV_COLS])
    cmask_d = din("cmask", [2, 128, 128], U8)
    cpool_d = din("cpool", [12, 128, 128])
    csel_d = din("csel", [32, NEXP * 128])
    cident_d = din("cident", [128, 128])
    out = nc.dram_tensor("out", [2, SEQ, D], F32, kind="ExternalOutput").ap()
    dbg = None
    if debug:
        dbg = nc.dram_tensor("dbg", [128, 8, TMAX], F32, kind="ExternalOutput").ap()

    P = Prog(nc)
    hT = P.sb("hT", [128, 8, TMAX], F32)
    hnT = P.sb("hnT", [128, 8, TMAX], BF16)
    yg = P.sb("yg", [128, 8, TMAX], BF16)
    NSLOT = 6
    wsl = [P.sb("wsl%d" % i, [128, 4096], BF16) for i in range(NSLOT)]
    ft = [P.sb("ft%d" % i, [128, 512], F32) for i in range(6)]
    bt = [P.sb("bt%d" % i, [128, 512], BF16) for i in range(12)]
    xin = P.sb("xin", [128, D], F32)
    ost = xin
    utok = P.sb("utok", [128, 5, D], BF16)
    uprev = [P.sb("uprev%d" % l, [128, D], BF16) for l in range(NL)]
    S32 = [P.sb("S32_%d" % l, [128, 4, 256], F32) for l in range(NL)]
    Sbf = [P.sb("Sbf_%d" % l, [128, 4, 256], BF16) for l in range(NL)]
    vtok = P.sb("vtok", [128, 8, 256], BF16)
    attn = [P.sb("attn%d" % i, [128, 128], BF16) for i in range(4)]
    kitok = [P.sb("kitok%d" % i, [128, 128], BF16) for i in range(4)]
    SbfT = P.sb("SbfT", [128, 3, 256], BF16)
    wglow = P.sb("wglow", [128, 8, 16], BF16)
    wgu_sb = P.sb("wgu_sb", [16, 512], BF16)
    wpool_sb = P.sb("wpool_sb", [128, 4, 2, 256], BF16)
    wrt_sb = P.sb("wrt_sb", [128, 8, 20], F32)
    pvec = P.sb("pvec_sb", [128, PV_COLS], F32)
    negb = P.sb("negb", [128, NL * 4], F32)
    ident_f = P.sb("ident_f", [128, 128], F32)
    ident_b = P.sb("ident_b", [128, 128], BF16)
    ones_b = P.sb("ones_b", [128, 128], BF16)
    ones_f = P.sb("ones_f", [128, 128], F32)
    cmask = P.sb("cmask_sb", [128, 2, 128], U8)
    cpool = P.sb("cpool_sb", [128, 12, 128], BF16)
    csel = P.sb("csel_sb", [32, NEXP * 128], BF16)
    combT = P.sb("combT", [32, TMAX], BF16)
    glowT = combT[0:16, :]
    RW = 32
    rt = [P.sb("rt%d" % i, [128, 9, RW if i == 7 else 20], F32) for i in range(8)]
    pb = [P.ps("pb%d" % i, [128, 512], F32) for i in range(7)]
    pbt = P.ps("pbt", [128, 1024], BF16)

    def tk(name, c, t0, n=1):
        return [(name, c, t) for t in range(t0, t0 + n)]

    def tk8(name, t0, n=1):
        return [(name, c, t) for c in range(8) for t in range(t0, t0 + n)]

    P.add("sp", I("dma_start", out=pvec[:], in_=pvec_d), writes=["pvec"], group="c0")
    P.add("sp", I("dma_start", out=ident_f[:], in_=cident_d), writes=["ident_f"], group="c0")
    P.add("sp", I("dma_start", out=cmask[:], in_=cmask_d.rearrange("a p n -> p a n")), writes=["cmask"], group="c0")
    P.add("pool", I("dma_start", out=ident_b[:], in_=cident_d), writes=["ident_b"], group="c1")
    P.add("pool", I("dma_start", out=cpool[:], in_=cpool_d.rearrange("a p n -> p a n")), writes=["cpool"], group="c1")
    P.add("pool", I("dma_start", out=csel[:], in_=csel_d), writes=["csel"], group="c1")
    P.add("dve", I("memset", ones_b[:], 1.0), writes=["ones_b"])
    P.add("dve", I("memset", ones_f[:], 1.0), writes=["ones_f"])
    for l in range(NL):
        P.add("dve", I("tensor_scalar", out=negb[:, l * 4:(l + 1) * 4], in0=pvec[:, l * PV_L + PV_BG:l * PV_L + PV_BG + 4],
                                                    scalar1=-1.0, scalar2=None, op0=ALU.mult),
              reads=["pvec"], writes=[("negb", l)])

    slot_rr = [0]

    def wload(dram_ap, shape3, nm):
        s = slot_rr[0] % NSLOT
        slot_rr[0] += 1
        a, b = shape3
        view = wsl[s][:, 0:a * b].rearrange("p (a b) -> p a b", a=a)
        key = ("wsl", s)
        P.add("pool", I("dma_start", out=view, in_=dram_ap), writes=[key], group="w%d" % s, name=nm)
        return view, key

    def w_in_cols(l, c0, n):
        return w_in[l].rearrange("(kc p) n -> p kc n", p=128)[:, :, c0:c0 + n]

    def w_sq(wd, l, half):
        return wd[l].rearrange("(kc p) n -> p kc n", p=128)[:, :, half * 512:(half + 1) * 512]

    def norm_ntile(l_col, a, n, t0, ntl, dst_fn, dst_keys_fn):
        for c in range(8):
            P.add("act", I("activation", out=bt[c][:, 0:n], in_=hT[:, c, a:a + n], func=AF.Square),
                  reads=tk("h", c, t0, ntl), writes=[("bt", c)])
        for c in range(8):
            P.add("pe", I("matmul", pb[2][:, 0:n], lhsT=ones_b[:], rhs=bt[c][:, 0:n], start=(c == 0), stop=(c == 7)),
                  reads=[("bt", c), "ones_b"], writes=[("pb", 2)])
        P.add("act", I("activation", out=ft[0][:, 0:n], in_=pb[2][:, 0:n], func=AF.Ln, scale=1.0 / D, bias=EPS),
              reads=[("pb", 2)], writes=[("ft", 0)])
        P.add("act", I("activation", out=ft[1][:, 0:n], in_=ft[0][:, 0:n], func=AF.Exp, scale=-0.5),
              reads=[("ft", 0)], writes=[("ft", 1)])
        for c in range(8):
            P.add("dve", I("scalar_tensor_tensor", out=dst_fn(c), in0=hT[:, c, a:a + n], scalar=pvec[:, l_col + c:l_col + c + 1],
                                                               in1=ft[1][:, 0:n], op0=ALU.mult, op1=ALU.mult),
                  reads=tk("h", c, t0, ntl) + [("ft", 1), "pvec"], writes=dst_keys_fn(c))

    for seq in range(n_seq):
        for gi, (g0, gn) in enumerate(groups):
            T = gn * 128
            NTS = ntiles_of(gn)
            c_lo = 112 if g0 == 0 else 0
            c_tot = T - c_lo
            npart = (c_tot + 511) // 512
            CNT = []
            ca = c_lo
            for pi in range(npart):
                cn = c_tot // npart + (1 if pi < c_tot % npart else 0)
                CNT.append((ca, cn, ca // 128, (ca + cn - 1) // 128 - ca // 128 + 1))
                ca += cn
            P.phase = 'load'
            for tl in range(gn):
                gt = g0 + tl
                alt = (tl % 2 == 1)
                if gt == 0:
                    P.add("dve", I("memset", xin[:], 0.0), writes=["xin"])
                    P.add("sp", I("dma_start", out=xin[112:128, :], in_=meta), writes=["xin"], group="xin")
                elif not alt:
                    P.add("sp", I("dma_start", out=xin[:], in_=x[seq, (gt - 1) * 128:gt * 128, :]), writes=["xin"], group="xin")
                else:
                    for hh in range(2):
                        P.add("sp", I("dma_start", out=ft[hh][:], in_=x[seq, (gt - 1) * 128:gt * 128, hh * 512:(hh + 1) * 512]), writes=[("ft", hh)], group="xin2")
                for c in range(8):
                    b = c // 4
                    if alt and gt != 0:
                        src_ap, src_k = ft[c // 4][:, (c % 4) * 128:(c % 4 + 1) * 128], ("ft", c // 4)
                    else:
                        src_ap, src_k = xin[:, c * 128:(c + 1) * 128], "xin"
                    P.add("pe", I("transpose", out=pb[b][:, (c % 4) * 128:(c % 4 + 1) * 128], in_=src_ap, identity=ident_f[:]),
                          reads=[src_k, "ident_f"], writes=[("pb", b)])
                for b in range(2):
                    P.add("act", I("activation", out=hT[:, b * 4:(b + 1) * 4, tl * 128:(tl + 1) * 128],
                                                                   in_=pb[b][:].rearrange("p (c t) -> p c t", c=4), func=AF.Copy),
                          reads=[("pb", b)], writes=[("h", c, tl) for c in range(b * 4, b * 4 + 4)])
            if g0 == 0:
                for l in range(NL):
                    P.add("pool", I("memset", S32[l][:], 0.0), writes=[("S32", l, h) for h in range(4)])
                    P.add("pool", I("memset", Sbf[l][:], 0.0), writes=[("Sbf", l, h) for h in range(4)])
                    P.add("pool", I("memset", uprev[l][:], 0.0), writes=[("uprev", l)])
                for i in range(4):
                    P.add("pool", I("memset", attn[i][:], 0.0), writes=[("attn", i)])

            for l in range(n_layers):
                pl = l * PV_L
                P.add("pool", I("dma_start", out=wglow[:], in_=w_in_cols(l, C_GL, 16)), writes=["wglow"], group="ws")
                P.add("pool", I("dma_start", out=wgu_sb[:], in_=w_gu[l]), writes=["wgu"], group="ws")
                P.add("pool", I("dma_start", out=wpool_sb[:], in_=w_pool[l].rearrange("g (kc p) n -> p g kc n", p=128)), writes=["wpool"], group="ws")
                P.add("sp", I("dma_start", out=wrt_sb[:], in_=w_rt[l].rearrange("(kc p) n -> p kc n", p=128)), writes=["wrt"], group="ws2")

                P.phase = 'norm1'
                for (t0, ntl) in NTS:
                    a, n = t0 * 128, ntl * 128
                    norm_ntile(pl + PV_N1, a, n, t0, ntl, lambda c, a=a, n=n: hnT[:, c, a:a + n], lambda c, t0=t0, ntl=ntl: tk("hn", c, t0, ntl))

                for (t0, ntl) in NTS:
                    a, n = t0 * 128, ntl * 128
                    for kc in range(8):
                        P.add("pe", I("matmul", pb[2][0:16, 0:n], lhsT=wglow[:, kc, :], rhs=hnT[:, kc, a:a + n], start=(kc == 0), stop=(kc == 7)),
                              reads=["wglow"] + tk("hn", kc, t0, ntl), writes=[("pb", 2)])
                    P.add("act", I("activation", out=glowT[:, a:a + n], in_=pb[2][0:16, 0:n], func=AF.Copy),
                          reads=[("pb", 2)], writes=tk("combT", 0, t0, ntl))

                P.phase = 'gla'
                items = [(hd, ni) for hd in (range(4) if 'gla' in phases else []) for ni in range(len(NTS))]
                hw = {}

                def head_weights(hd):
                    if hd in hw:
                        return hw[hd]
                    s = slot_rr[0] % NSLOT
                    slot_rr[0] += 1
                    va = wsl[s][:, 0:4096].rearrange("p (a b) -> p a b", a=8)
                    key = ("wsl", s)
                    for (c0, ncol, off) in ((C_Q + hd * 128, 128, 0), (C_K + hd * 128, 128, 128), (C_V + hd * 256, 256, 256)):
                        P.add("pool", I("dma_start", out=va[:, :, off:off + ncol], in_=w_in_cols(l, c0, ncol)), writes=[key], group="w%d" % s, name="wqkv")
                    wog, kog = wload(w_in_cols(l, C_OG + hd * 256, 256), (8, 256), "wog")
                    hw[hd] = (va, key, wog, kog)
                    return hw[hd]

                def gla_p1(i):
                    hd, ni = items[i]
                    par = i % 2
                    t0, ntl = NTS[ni]
                    a, n = t0 * 128, ntl * 128
                    va, kva, wog, kog = head_weights(hd)
                    B = [bt[0], bt[1], bt[2], bt[3]] if par == 0 else [bt[8], bt[9], bt[10], bt[11]]
                    Bk = [("bt", 0), ("bt", 1), ("bt", 2), ("bt", 3)] if par == 0 else [("bt", 8), ("bt", 9), ("bt", 10), ("bt", 11)]
                    eB, keB = ft[2 + par], ("ft", 2 + par)
                    hnk = lambda kc: tk("hn", kc, t0, ntl)
                    P.phase = 'gla.chain'
                    P.add("pe", I("matmul", pb[2][:, 0:n], lhsT=wgu_sb[:, hd * 128:(hd + 1) * 128], rhs=glowT[:, a:a + n], start=True, stop=True),
                          reads=["wgu"] + tk("combT", 0, t0, ntl), writes=[("pb", 2)])
                    P.add("act", I("activation", out=ft[0][:, 0:n], in_=pb[2][:, 0:n], func=AF.Exp, scale=-1.0, bias=negb[:, l * 4 + hd:l * 4 + hd + 1]),
                          reads=[("pb", 2), ("negb", l)], writes=[("ft", 0)])
                    P.add("act", I("activation", out=ft[0][:, 0:n], in_=ft[0][:, 0:n], func=AF.Ln, bias=1.0),
                          reads=[("ft", 0)], writes=[("ft", 0)])
                    if g0 + t0 == 0:
                        P.add("dve", I("memset", ft[0][:, 0:112], 0.0), reads=[("ft", 0)], writes=[("ft", 0)])
                    for j in range(ntl):
                        P.add("dve", I("tensor_tensor_scan", out=ft[1][:, j * 128:(j + 1) * 128], data0=ones_f[:], data1=ft[0][:, j * 128:(j + 1) * 128],
                                       initial=0.0, op0=ALU.mult, op1=ALU.add),
                              reads=[("ft", 0), "ones_f"], writes=[("ft", 1)])
                    P.add("act", I("activation", out=eB[:, 0:n], in_=ft[1][:, 0:n], func=AF.Exp, scale=-1.0 / 16.0),
                          reads=[("ft", 1)], writes=[keB])
                    P.add("act", I("activation", out=ft[0][:, 0:n], in_=ft[1][:, 0:n], func=AF.Exp, scale=1.0 / 16.0),
                          reads=[("ft", 1)], writes=[("ft", 0)])
                    P.phase = 'gla.qk'
                    for kc in range(8):
                        P.add("pe", I("matmul", pb[0][:, 0:n], lhsT=va[:, kc, 0:128], rhs=hnT[:, kc, a:a + n], start=(kc == 0), stop=(kc == 7)),
                              reads=[kva] + hnk(kc), writes=[("pb", 0)])
                    for kc in range(8):
                        P.add("pe", I("matmul", pb[1][:, 0:n], lhsT=va[:, kc, 128:256], rhs=hnT[:, kc, a:a + n], start=(kc == 0), stop=(kc == 7)),
                              reads=[kva] + hnk(kc), writes=[("pb", 1)])
                    P.add("dve", I("scalar_tensor_tensor", out=B[0][:, 0:n], in0=pb[0][:, 0:n], scalar=QSCALE, in1=eB[:, 0:n], op0=ALU.mult, op1=ALU.mult),
                          reads=[("pb", 0), keB], writes=[Bk[0]])
                    P.add("dve", I("scalar_tensor_tensor", out=B[1][:, 0:n], in0=pb[0][:, 0:n], scalar=QSCALE, in1=ft[0][:, 0:n], op0=ALU.mult, op1=ALU.mult),
                          reads=[("pb", 0), ("ft", 0)], writes=[Bk[1]])
                    P.add("dve", I("tensor_tensor", out=B[2][:, 0:n], in0=pb[1][:, 0:n], in1=ft[0][:, 0:n], op=ALU.mult),
                          reads=[("pb", 1), ("ft", 0)], writes=[Bk[2]])
                    P.add("dve", I("tensor_tensor", out=B[3][:, 0:n], in0=pb[1][:, 0:n], in1=eB[:, 0:n], op=ALU.mult),
                          reads=[("pb", 1), keB], writes=[Bk[3]])
                    P.phase = 'gla.v'
                    for j in range(ntl):
                        jj = j % 2
                        for kc in range(8):
                            P.add("pe", I("matmul", pb[3 + jj][:, 0:256], lhsT=hnT[:, kc, a + j * 128:a + (j + 1) * 128], rhs=va[:, kc, 256:512],
                                          start=(kc == 0), stop=(kc == 7)),
                                  reads=[kva] + tk("hn", kc, t0 + j), writes=[("pb", 3 + jj)])
                        P.add("act", I("activation", out=vtok[:, par * 4 + j, :], in_=pb[3 + jj][:, 0:256], func=AF.Copy),
                              reads=[("pb", 3 + jj)], writes=[("vtok", par * 4 + j)])

                def gla_s(i):
                    hd, ni = items[i]
                    par = i % 2
                    t0, ntl = NTS[ni]
                    a, n = t0 * 128, ntl * 128
                    va, kva, wog, kog = head_weights(hd)
                    B = [bt[0], bt[1], bt[2], bt[3]] if par == 0 else [bt[8], bt[9], bt[10], bt[11]]
                    Bk = [("bt", 0), ("bt", 1), ("bt", 2), ("bt", 3)] if par == 0 else [("bt", 8), ("bt", 9), ("bt", 10), ("bt", 11)]
                    eB, keB = ft[2 + par], ("ft", 2 + par)
                    hnk = lambda kc: tk("hn", kc, t0, ntl)
                    def og_c(c):
                        P.phase = 'gla.og'
                        for kc in range(8):
                            P.add("pe", I("matmul", pb[c][:, 0:n], lhsT=wog[:, kc, c * 128:(c + 1) * 128], rhs=hnT[:, kc, a:a + n], start=(kc == 0), stop=(kc == 7)),
                                  reads=[kog] + hnk(kc), writes=[("pb", c)])
                        sg = ft[4 + c]
                        P.add("act", I("activation", out=sg[:, 0:n], in_=pb[c][:, 0:n], func=AF.Exp, scale=-1.0),
                              reads=[("pb", c)], writes=[("ft", 4 + c)])
                        P.add("act", I("activation", out=sg[:, 0:n], in_=sg[:, 0:n], func=AF.Ln, bias=1.0),
                              reads=[("ft", 4 + c)], writes=[("ft", 4 + c)])
                        P.add("act", I("activation", out=sg[:, 0:n], in_=sg[:, 0:n], func=AF.Exp, scale=-1.0),
                              reads=[("ft", 4 + c)], writes=[("ft", 4 + c)])
                        P.add("dve", I("tensor_tensor", out=bt[4 + c][:, 0:n], in0=pb[c][:, 0:n], in1=sg[:, 0:n], op=ALU.mult),
                              reads=[("pb", c), ("ft", 4 + c)], writes=[("bt", 4 + c)])

                    P.phase = 'gla.scan'
                    for j in range(ntl):
                        P.add("pe", I("transpose", out=pbt[:, j * 128:(j + 1) * 128], in_=B[2][:, j * 128:(j + 1) * 128], identity=ident_b[:]),
                              reads=[Bk[2], "ident_b"], writes=[("pbt", 0)])
                    for j in range(ntl):
                        P.add("act", I("activation", out=kitok[j][:], in_=pbt[:, j * 128:(j + 1) * 128], func=AF.Copy),
                              reads=[("pbt", 0)], writes=[("kitok", j)])
                    P.phase = 'gla.scan'
                    for j in range(ntl):
                        vj = par * 4 + j
                        ja, jb = j * 128, (j + 1) * 128
                        bA = 4 - (j % 2)
                        pA = pb[bA]
                        P.add("pe", I("matmul", pA[:, 0:128], lhsT=B[2][:, ja:jb], rhs=B[0][:, ja:jb], start=True, stop=True),
                              reads=[Bk[2], Bk[0]], writes=[("pb", bA)])
                        P.add("pe", I("matmul", pA[:, 128:256], lhsT=B[3][:, ja:jb], rhs=B[1][:, ja:jb], start=True, stop=True),
                              reads=[Bk[3], Bk[1]], writes=[("pb", bA)])
                        P.add("dve", I("copy_predicated", out=attn[j][:], mask=cmask[:, 0, :], data=pA[:, 0:128]),
                              reads=[("pb", bA), "cmask"], writes=[("attn", j)])
                        P.add("dve", I("copy_predicated", out=attn[j][:], mask=cmask[:, 1, :], data=pA[:, 128:256]),
                              reads=[("pb", bA), "cmask"], writes=[("attn", j)])
                        if j == min(1, ntl - 1):
                            og_c(0)
                            P.phase = 'gla.scan'
                    og_c(1)
                    P.phase = 'gla.scan'
                    for j in range(ntl):
                        vj = par * 4 + j
                        ja, jb = j * 128, (j + 1) * 128
                        bA = 4 - (j % 2)
                        pA = pb[bA]
                        P.add("pe", I("matmul", pA[:, 256:512], lhsT=kitok[j][:], rhs=vtok[:, vj, :], start=True, stop=True),
                              reads=[("kitok", j), ("vtok", vj)], writes=[("pb", bA)])
                        P.add("act", I("activation", out=ft[4 + j // 2][:, (j % 2) * 256:(j % 2) * 256 + 256], in_=pA[:, 256:512], func=AF.Identity, scale=eB[:, jb - 1:jb]),
                              reads=[("pb", bA), keB], writes=[("ft", 4 + j // 2)])
                    P.phase = 'gla.seq'
                    for j in range(ntl):
                        vj = par * 4 + j
                        ja, jb = j * 128, (j + 1) * 128
                        if j == 0:
                            Sprev, kSprev = Sbf[l][:, hd, :], ("Sbf", l, hd)
                        else:
                            Sprev, kSprev = SbfT[:, j - 1, :], ("SbfT", j - 1)
                        for c in range(2):
                            P.add("pe", I("matmul", pb[5 + c][:, ja:jb], lhsT=vtok[:, vj, c * 128:(c + 1) * 128], rhs=attn[j][:], start=True, stop=False),
                                  reads=[("vtok", vj), ("attn", j)], writes=[("pb", 5 + c)])
                            P.add("pe", I("matmul", pb[5 + c][:, ja:jb], lhsT=Sprev[:, c * 128:(c + 1) * 128], rhs=B[0][:, ja:jb], start=False, stop=True),
                                  reads=[kSprev, Bk[0]], writes=[("pb", 5 + c)])
                        if j == ntl - 1:
                            Snext, kSnext = Sbf[l][:, hd, :], ("Sbf", l, hd)
                        else:
                            Snext, kSnext = SbfT[:, j, :], ("SbfT", j)
                        P.add("dve", I("scalar_tensor_tensor", out=Snext, in0=S32[l][:, hd, :], scalar=eB[:, jb - 1:jb],
                                       in1=ft[4 + j // 2][:, (j % 2) * 256:(j % 2) * 256 + 256], op0=ALU.mult, op1=ALU.add),
                              reads=[("S32", l, hd), keB, ("ft", 4 + j // 2)], writes=[kSnext])
                        P.add("dve", I("scalar_tensor_tensor", out=S32[l][:, hd, :], in0=S32[l][:, hd, :], scalar=eB[:, jb - 1:jb],
                                       in1=ft[4 + j // 2][:, (j % 2) * 256:(j % 2) * 256 + 256], op0=ALU.mult, op1=ALU.add),
                              reads=[("S32", l, hd), keB, ("ft", 4 + j // 2)], writes=[("S32", l, hd)])
                    P.phase = 'gla.fin'
                    for c in range(2):
                        P.add("act", I("activation", out=bt[6 + c][:, 0:n], in_=pb[5 + c][:, 0:n], func=AF.Square),
                              reads=[("pb", 5 + c)], writes=[("bt", 6 + c)])
                    for c in range(2):
                        P.add("pe", I("matmul", pb[4][:, 0:n], lhsT=ones_b[:], rhs=bt[6 + c][:, 0:n], start=(c == 0), stop=(c == 1)),
                              reads=[("bt", 6 + c), "ones_b"], writes=[("pb", 4)])
                    P.add("act", I("activation", out=ft[4][:, 0:n], in_=pb[4][:, 0:n], func=AF.Ln, scale=1.0 / 256.0, bias=EPS),
                          reads=[("pb", 4)], writes=[("ft", 4)])
                    P.add("act", I("activation", out=ft[4][:, 0:n], in_=ft[4][:, 0:n], func=AF.Exp, scale=-0.5),
                          reads=[("ft", 4)], writes=[("ft", 4)])
                    for c in range(2):
                        P.add("dve", I("scalar_tensor_tensor", out=ft[5][:, 0:n], in0=pb[5 + c][:, 0:n], scalar=pvec[:, pl + PV_GN + c:pl + PV_GN + c + 1],
                                       in1=ft[4][:, 0:n], op0=ALU.mult, op1=ALU.mult),
                              reads=[("pb", 5 + c), ("ft", 4), "pvec"], writes=[("ft", 5)])
                        P.add("dve", I("tensor_tensor", out=yg[:, hd * 2 + c, a:a + n], in0=ft[5][:, 0:n], in1=bt[4 + c][:, 0:n], op=ALU.mult),
                              reads=[("ft", 5), ("bt", 4 + c)], writes=tk("yg", hd * 2 + c, t0, ntl))

                if items:
                    gla_p1(0)
                for i in range(len(items)):
                    if i + 1 < len(items):
                        gla_p1(i + 1)
                    gla_s(i)

                P.phase = 'br0'
                def branch(wbr_d, ccol, src, srcname):
                    wb, wg = [], []
                    for hf in range(2):
                        wb.append(wload(w_sq(wbr_d, l, hf), (8, 512), "wbr"))
                        wg.append(wload(w_in_cols(l, ccol + hf * 512, 512), (8, 512), "wgate"))
                    wo = [wload(w_sq(w_out, l, hf), (8, 512), "wout") for hf in range(2)]
                    for (a, n, t0, ntl) in CNT:
                        for m in range(8):
                            hf, mo = m // 4, (m % 4) * 128
                            pbr, pgt = pb[(m % 2) * 2], pb[(m % 2) * 2 + 1]
                            for kc in range(8):
                                P.add("pe", I("matmul", pbr[:, 0:n], lhsT=wb[hf][0][:, kc, mo:mo + 128], rhs=src[:, kc, a:a + n], start=(kc == 0), stop=(kc == 7)),
                                      reads=[wb[hf][1]] + tk(srcname, kc, t0, ntl), writes=[("pb", (m % 2) * 2)])
                            for kc in range(8):
                                P.add("pe", I("matmul", pgt[:, 0:n], lhsT=wg[hf][0][:, kc, mo:mo + 128], rhs=hnT[:, kc, a:a + n], start=(kc == 0), stop=(kc == 7)),
                                      reads=[wg[hf][1]] + tk("hn", kc, t0, ntl), writes=[("pb", (m % 2) * 2 + 1)])
                            P.add("act", I("activation", out=ft[m % 2][:, 0:n], in_=pgt[:, 0:n], func=AF.Sigmoid),
                                  reads=[("pb", (m % 2) * 2 + 1)], writes=[("ft", m % 2)])
                            P.add("dve", I("tensor_tensor", out=bt[m][:, 0:n], in0=pbr[:, 0:n], in1=ft[m % 2][:, 0:n], op=ALU.mult),
                                  reads=[("pb", (m % 2) * 2), ("ft", m % 2)], writes=[("bt", m)])
                        for m in range(8):
                            hf, mo = m // 4, (m % 4) * 128
                            po = pb[4 + m % 2]
                            for kc in range(8):
                                P.add("pe", I("matmul", po[:, 0:n], lhsT=wo[hf][0][:, kc, mo:mo + 128], rhs=bt[kc][:, 0:n], start=(kc == 0), stop=(kc == 7)),
                                      reads=[wo[hf][1], ("bt", kc)], writes=[("pb", 4 + m % 2)])
                            P.add("dve", I("tensor_tensor", out=hT[:, m, a:a + n], in0=po[:, 0:n], in1=hT[:, m, a:a + n], op=ALU.add),
                                  reads=[("pb", 4 + m % 2)] + tk("h", m, t0, ntl), writes=tk("h", m, t0, ntl))

                if 'br0' in phases:
                    branch(w_brg, C_G0, yg, "yg")

                P.phase = 'pool'
                wu = [wload(w_in_cols(l, C_U + hf * 512, 512), (8, 512), "wu") for hf in range(2)]
                P.add("pool", I("tensor_copy", out=utok[:, 0, :], in_=uprev[l][:]), reads=[("uprev", l)], writes=[("utok", 0)])
                for (t0, ntl) in NTS:
                    a, n = t0 * 128, ntl * 128
                    for j in range(ntl):
                        for hf in range(2):
                            pu = pb[hf]
                            for kc in range(8):
                                P.add("pe", I("matmul", pu[:, 0:512], lhsT=hnT[:, kc, a + j * 128:a + (j + 1) * 128], rhs=wu[hf][0][:, kc, :], start=(kc == 0), stop=(kc == 7)),
                                      reads=[wu[hf][1]] + tk("hn", kc, t0 + j), writes=[("pb", hf)])
                            P.add("act", I("activation", out=utok[:, j + 1, hf * 512:(hf + 1) * 512], in_=pu[:, 0:512], func=AF.Copy),
                                  reads=[("pb", hf)], writes=[("utok", j + 1, hf)])
                    for g in range(4):
                        for cc in range(2):
                            c = g * 2 + cc
                            pp = pb[2 + c % 2]
                            for j in range(ntl):
                                first = (g0 + t0 + j == 0)
                                pcur = cpool[:, (8 + g) if first else g, :]
                                P.add("pe", I("matmul", pp[:, j * 128:(j + 1) * 128], lhsT=utok[:, j, c * 128:(c + 1) * 128], rhs=cpool[:, 4 + g, :], start=True, stop=False),
                                      reads=[("utok", j, 0), ("utok", j, 1), ("utok", j), "cpool"], writes=[("pb", 2 + c % 2)])
                                P.add("pe", I("matmul", pp[:, j * 128:(j + 1) * 128], lhsT=utok[:, j + 1, c * 128:(c + 1) * 128], rhs=pcur, start=False, stop=True),
                                      reads=[("utok", j + 1, 0), ("utok", j + 1, 1), ("utok", j + 1), "cpool"], writes=[("pb", 2 + c % 2)])
                            P.add("act", I("activation", out=bt[8 + c % 4][:, 0:n], in_=pp[:, 0:n], func=AF.Copy),
                                  reads=[("pb", 2 + c % 2)], writes=[("bt", 8 + c % 4)])
                        for oc in range(2):
                            pm = pb[4 + oc]
                            for cc in range(2):
                                c = g * 2 + cc
                                P.add("pe", I("matmul", pm[:, 0:n], lhsT=wpool_sb[:, g, cc, oc * 128:(oc + 1) * 128], rhs=bt[8 + c % 4][:, 0:n], start=(cc == 0), stop=(cc == 1)),
                                      reads=["wpool", ("bt", 8 + c % 4)], writes=[("pb", 4 + oc)])
                            mo = g * 2 + oc
                            P.add("act", I("activation", out=yg[:, mo, a:a + n], in_=pm[:, 0:n], func=AF.Identity, scale=pvec[:, pl + PV_PS + mo:pl + PV_PS + mo + 1]),
                                  reads=[("pb", 4 + oc), "pvec"], writes=tk("yg", mo, t0, ntl))
                    P.add("pool", I("tensor_copy", out=utok[:, 0, :], in_=utok[:, ntl, :]),
                          reads=[("utok", ntl, 0), ("utok", ntl, 1), ("utok", ntl)], writes=[("utok", 0)])
                P.add("pool", I("tensor_copy", out=uprev[l][:], in_=utok[:, 0, :]), reads=[("utok", 0)], writes=[("uprev", l)])

                P.phase = 'br1'
                if 'br1' in phases:
                    branch(w_brp, C_G1, yg, "yg")

                if debug == ("mix", l) and seq == 0 and gi == 0:
                    P.add("sp", I("dma_start", out=dbg[:, :, 0:T], in_=hT[:, :, 0:T]), reads=tk8("h", 0, gn), group="dbg")

                P.phase = 'norm2'
                for c in range(8):
                    P.add("dve", I("tensor_scalar", out=wrt_sb[:, c, :], in0=wrt_sb[:, c, :], scalar1=pvec[:, pl + PV_N2 + c:pl + PV_N2 + c + 1], scalar2=None, op0=ALU.mult),
                          reads=["wrt", "pvec"], writes=["wrt"])
                for tl in range(gn):
                    a = tl * 128
                    bi = 3 + tl % 4
                    for c in range(8):
                        P.add("pe", I("matmul", pb[bi][:, 0:20], lhsT=hT[:, c, a:a + 128], rhs=wrt_sb[:, c, :], start=(c == 0), stop=(c == 7)),
                              reads=tk("h", c, tl) + ["wrt"], writes=[("pb", bi)])
                    P.add("act", I("activation", out=rt[0][:, tl, 0:20], in_=pb[bi][:, 0:20], func=AF.Copy),
                          reads=[("pb", bi)], writes=[("rt", 0)])
                for (t0, ntl) in NTS:
                    a, n = t0 * 128, ntl * 128
                    norm_ntile(pl + PV_N2, a, n, t0, ntl, lambda c, a=a, n=n: hnT[:, c, a:a + n], lambda c, t0=t0, ntl=ntl: tk("hn", c, t0, ntl))
                    for j in range(ntl):
                        P.add("pe", I("matmul", pb[1][:, t0 + j:t0 + j + 1], lhsT=ft[1][0:1, j * 128:(j + 1) * 128], rhs=ones_f[0:1, 0:1], start=True, stop=True),
                              reads=[("ft", 1), "ones_f"], writes=[("pb", 1)])
                P.add("act", I("activation", out=rt[6][:, 0:gn, 19], in_=pb[1][:, 0:gn], func=AF.Copy),
                      reads=[("pb", 1)], writes=[("rt", 6)])
                P.add("dve", I("tensor_tensor", out=rt[0][:, 0:gn, 0:20], in0=rt[0][:, 0:gn, 0:20],
                               in1=rt[6][:, 0:gn, 19].unsqueeze(2).broadcast_to([128, gn, 20]), op=ALU.mult),
                      reads=[("rt", 0), ("rt", 6)], writes=[("rt", 0)])

                P.phase = 'route'
                G = gn
                Lg = lambda t: t[:, 0:G, 0:4]
                Le4 = rt[0][:, 0:G, 4:20].rearrange("p g (a b) -> p g a b", a=4)
                Le4T = rt[0][:, 0:G, 4:20].rearrange("p g (a b) -> p g b a", a=4)

                def dv(fn, reads, writes):
                    P.add("dve", fn, reads=[("rt", i) for i in reads], writes=[("rt", i) for i in writes])

                def bc(ap2, k):
                    return ap2.unsqueeze(2).broadcast_to([128, G, k])

                mg = rt[1][:, 0:G, 0]
                dv(I("tensor_reduce", out=mg, in_=Lg(rt[0]), axis=mybir.AxisListType.X, op=ALU.max), [0], [1])
                dv(I("tensor_tensor", out=rt[1][:, 0:G, 4:8], in0=Lg(rt[0]), in1=bc(mg, 4), op=ALU.is_equal), [0, 1], [1])
                dv(I("tensor_tensor", out=rt[2][:, 0:G, 0:4], in0=Lg(rt[0]), in1=bc(mg, 4), op=ALU.subtract), [0, 1], [2])
                P.add("act", I("activation", out=rt[2][:, 0:G, 0:4], in_=rt[2][:, 0:G, 0:4], func=AF.Exp), reads=[("rt", 2)], writes=[("rt", 2)])
                dv(I("tensor_reduce", out=rt[2][:, 0:G, 4], in_=rt[2][:, 0:G, 0:4], axis=mybir.AxisListType.X, op=ALU.add), [2], [2])
                dv(I("tensor_tensor", out=rt[3][:, 0:G, 0:16].rearrange("p g (a b) -> p g a b", a=4), in0=Le4,
                                             in1=rt[1][:, 0:G, 4:8].unsqueeze(3).broadcast_to([128, G, 4, 4]), op=ALU.mult), [0, 1], [3])
                dv(I("tensor_reduce", out=rt[3][:, 0:G, 16:20], in_=rt[3][:, 0:G, 0:16].rearrange("p g (a b) -> p g b a", a=4), axis=mybir.AxisListType.X, op=ALU.add), [3], [3])
                les = lambda: rt[3][:, 0:G, 16:20]
                m1 = rt[4][:, 0:G, 0]
                dv(I("tensor_reduce", out=m1, in_=les(), axis=mybir.AxisListType.X, op=ALU.max), [3], [4])
                dv(I("tensor_tensor", out=rt[4][:, 0:G, 4:8], in0=les(), in1=bc(m1, 4), op=ALU.is_equal), [3, 4], [4])
                dv(I("scalar_tensor_tensor", out=rt[4][:, 0:G, 8:12], in0=rt[4][:, 0:G, 4:8], scalar=-1e30, in1=les(), op0=ALU.mult, op1=ALU.add), [3, 4], [4])
                m2 = rt[4][:, 0:G, 1]
                dv(I("tensor_reduce", out=m2, in_=rt[4][:, 0:G, 8:12], axis=mybir.AxisListType.X, op=ALU.max), [4], [4])
                dv(I("tensor_tensor", out=rt[4][:, 0:G, 12:16], in0=rt[4][:, 0:G, 8:12], in1=bc(m2, 4), op=ALU.is_equal), [4], [4])
                dv(I("tensor_tensor", out=rt[5][:, 0:G, 0], in0=m2, in1=m1, op=ALU.subtract), [4], [5])
                P.add("act", I("activation", out=rt[5][:, 0:G, 0], in_=rt[5][:, 0:G, 0], func=AF.Exp), reads=[("rt", 5)], writes=[("rt", 5)])
                dv(I("scalar_tensor_tensor", out=rt[5][:, 0:G, 1], in0=rt[5][:, 0:G, 0], scalar=1.0, in1=rt[2][:, 0:G, 4], op0=ALU.add, op1=ALU.mult), [5, 2], [5])
                dv(I("reciprocal", out=rt[5][:, 0:G, 2], in_=rt[5][:, 0:G, 1]), [5], [5])
                dv(I("tensor_tensor", out=rt[5][:, 0:G, 3], in0=rt[5][:, 0:G, 2], in1=rt[5][:, 0:G, 0], op=ALU.mult), [5], [5])
                dv(I("tensor_tensor", out=rt[5][:, 0:G, 4:8], in0=rt[4][:, 0:G, 4:8], in1=bc(rt[5][:, 0:G, 2], 4), op=ALU.mult), [4, 5], [5])
                dv(I("tensor_tensor", out=rt[5][:, 0:G, 8:12], in0=rt[4][:, 0:G, 12:16], in1=bc(rt[5][:, 0:G, 3], 4), op=ALU.mult), [4, 5], [5])
                dv(I("tensor_tensor", out=rt[5][:, 0:G, 4:8], in0=rt[5][:, 0:G, 4:8], in1=rt[5][:, 0:G, 8:12], op=ALU.add), [5], [5])
                dv(I("tensor_tensor", out=rt[6][:, 0:G, 0:16].rearrange("p g (a b) -> p g a b", a=4),
                                             in0=rt[1][:, 0:G, 4:8].unsqueeze(3).broadcast_to([128, G, 4, 4]),
                                             in1=rt[5][:, 0:G, 4:8].unsqueeze(2).broadcast_to([128, G, 4, 4]), op=ALU.mult), [1, 5], [6])
                P.add("dve", I("tensor_copy", out=bt[0][:, 0:G * 16].rearrange("p (g k) -> p g k", g=G), in_=rt[6][:, 0:G, 0:16]),
                      reads=[("rt", 6)], writes=[("bt", 0)])
                P.add("dve", I("tensor_copy", out=rt[7][:, 0:G, 0:16], in_=bt[0][:, 0:G * 16].rearrange("p (g k) -> p g k", g=G)),
                      reads=[("bt", 0)], writes=[("rt", 7)])
                dv(I("tensor_tensor", out=rt[7][:, 0:G, 16:32], in0=rt[6][:, 0:G, 0:16], in1=rt[7][:, 0:G, 0:16], op=ALU.subtract), [6, 7], [7])
                for tl in range(gn):
                    P.add("pe", I("transpose", out=pb[3][0:32, (tl % 4) * 128:(tl % 4 + 1) * 128], in_=rt[7][:, tl, 0:32], identity=ident_f[:]),
                          reads=[("rt", 7), "ident_f"], writes=[("pb", 3)])
                    P.add("act", I("activation", out=combT[:, tl * 128:(tl + 1) * 128], in_=pb[3][0:32, (tl % 4) * 128:(tl % 4 + 1) * 128], func=AF.Copy),
                          reads=[("pb", 3)], writes=tk("combT", 0, tl))

                P.phase = 'moe'
                blocks = [(ex, ni) for ex in (range(NEXP) if 'moe' in phases else []) for ni in range(len(CNT))]
                ew = {}

                def exp_w(ex):
                    if ex not in ew:
                        ew[ex] = (wload(w_eg[l, ex].rearrange("(kc p) n -> p kc n", p=128), (8, 512), "weg"),
                                  wload(w_eu[l, ex].rearrange("(kc p) n -> p kc n", p=128), (8, 512), "weu"),
                                  wload(w_ed[l, ex].rearrange("(kc p) n -> p kc n", p=128), (4, 1024), "wed"))
                    return ew[ex]

                def moe_gu(b):
                    ex, ni = blocks[b]
                    a, n, t0, ntl = CNT[ni]
                    par = b % 2
                    (weg, keg), (weu, keu), (wed, ked) = exp_w(ex)
                    P.add("pe", I("matmul", pb[6][:, 0:n], lhsT=csel[:, ex * 128:(ex + 1) * 128], rhs=combT[:, a:a + n], start=True, stop=True),
                          reads=["csel"] + tk("combT", 0, t0, ntl), writes=[("pb", 6)])
                    P.add("act", I("activation", out=ft[par][:, 0:n], in_=pb[6][:, 0:n], func=AF.Copy),
                          reads=[("pb", 6)], writes=[("ft", par)])
                    for fc in range(4):
                        pg_, pu_ = pb[(fc % 2) * 2], pb[(fc % 2) * 2 + 1]
                        for kc in range(8):
                            P.add("pe", I("matmul", pg_[:, 0:n], lhsT=weg[:, kc, fc * 128:(fc + 1) * 128], rhs=hnT[:, kc, a:a + n], start=(kc == 0), stop=(kc == 7)),
                                  reads=[keg] + tk("hn", kc, t0, ntl), writes=[("pb", (fc % 2) * 2)])
                        for kc in range(8):
                            P.add("pe", I("matmul", pu_[:, 0:n], lhsT=weu[:, kc, fc * 128:(fc + 1) * 128], rhs=hnT[:, kc, a:a + n], start=(kc == 0), stop=(kc == 7)),
                                  reads=[keu] + tk("hn", kc, t0, ntl), writes=[("pb", (fc % 2) * 2 + 1)])
                        fs, fu = ft[2 + fc % 2], ft[4 + fc % 2]
                        P.add("act", I("activation", out=fs[:, 0:n], in_=pg_[:, 0:n], func=AF.Silu),
                              reads=[("pb", (fc % 2) * 2)], writes=[("ft", 2 + fc % 2)])
                        P.add("dve", I("tensor_tensor", out=fu[:, 0:n], in0=pu_[:, 0:n], in1=fs[:, 0:n], op=ALU.mult),
                              reads=[("pb", (fc % 2) * 2 + 1), ("ft", 2 + fc % 2)], writes=[("ft", 4 + fc % 2)])
                        P.add("dve", I("tensor_tensor", out=bt[par * 4 + fc][:, 0:n], in0=fu[:, 0:n], in1=ft[par][:, 0:n], op=ALU.mult),
                              reads=[("ft", 4 + fc % 2), ("ft", par)], writes=[("bt", par * 4 + fc)])

                def moe_dn(b):
                    ex, ni = blocks[b]
                    a, n, t0, ntl = CNT[ni]
                    par = b % 2
                    (weg, keg), (weu, keu), (wed, ked) = exp_w(ex)
                    for m in range(8):
                        bi = 4 + (b * 8 + m) % 3
                        pd = pb[bi]
                        for fc in range(4):
                            P.add("pe", I("matmul", pd[:, 0:n], lhsT=wed[:, fc, m * 128:(m + 1) * 128], rhs=bt[par * 4 + fc][:, 0:n], start=(fc == 0), stop=(fc == 3)),
                                  reads=[ked, ("bt", par * 4 + fc)], writes=[("pb", bi)])
                        P.add("dve", I("tensor_tensor", out=hT[:, m, a:a + n], in0=pd[:, 0:n], in1=hT[:, m, a:a + n], op=ALU.add),
                              reads=[("pb", bi)] + tk("h", m, t0, ntl), writes=tk("h", m, t0, ntl))

                if blocks:
                    moe_gu(0)
                for b in range(len(blocks)):
                    if b + 1 < len(blocks):
                        moe_gu(b + 1)
                    moe_dn(b)

                if debug == ("moe", l) and seq == 0 and gi == 0:
                    P.add("sp", I("dma_start", out=dbg[:, :, 0:T], in_=hT[:, :, 0:T]), reads=tk8("h", 0, gn), group="dbg")

            P.phase = 'final'
            tl = 1 if g0 == 0 else 0
            while tl < gn:
                ntl = min(2, gn - tl)
                a, n = tl * 128, ntl * 128
                norm_ntile(PV_FN, a, n, tl, ntl,
                           lambda c, n=n: ft[2 + c // 2][:, (c % 2) * 256:(c % 2) * 256 + n],
                           lambda c: [("ft", 2 + c // 2)])
                for j in range(ntl):
                    gt = g0 + tl + j
                    for c in range(8):
                        b = c // 4
                        o = (c % 2) * 256 + j * 128
                        P.add("pe", I("transpose", out=pb[b][:, (c % 4) * 128:(c % 4 + 1) * 128], in_=ft[2 + c // 2][:, o:o + 128], identity=ident_f[:]),
                              reads=[("ft", 2 + c // 2), "ident_f"], writes=[("pb", b)])
                    for b in range(2):
                        P.add("act", I("activation", out=ost[:, b * 512:(b + 1) * 512], in_=pb[b][:, 0:512], func=AF.Copy),
                              reads=[("pb", b)], writes=["xin"])
                    P.add("sp", I("dma_start", out=out[seq, (gt - 1) * 128:gt * 128, :], in_=ost[:]),
                          reads=["xin"], group="xin")
                tl += ntl

    P.emit()
    P.close()
    nc._prog_ops = P.ops
    return nc


def host_constants():
    j = np.arange(128)[:, None]
    i = np.arange(128)[None, :]
    m_lo = (i >= j).astype(np.uint8)
    m_up = ((i < j) & (i // 64 == j // 64)).astype(np.uint8)
    cmask = np.stack([m_lo, m_up]).astype(np.uint8)
    cpool = np.zeros((12, 128, 128), np.float32)
    s = np.arange(128)[:, None]
    t = np.arange(128)[None, :]
    for g, w in enumerate((2, 4, 8, 16)):
        dcur = t - s
        cpool[g] = np.where((dcur >= 0) & (dcur < w), 1.0 / w, 0.0) - (dcur == 0)
        dprev = t + 128 - s
        cpool[4 + g] = np.where(dprev < w, 1.0 / w, 0.0)
        tseq = t - 112
        cnt = np.minimum(tseq + 1, w).astype(np.float32)
        cnt = np.where(cnt > 0, cnt, 1.0)
        cpool[8 + g] = np.where((dcur >= 0) & (dcur < w) & (s >= 112), 1.0 / cnt, 0.0) - ((dcur == 0) & (s >= 112))
    csel = np.zeros((32, NEXP, 128), np.float32)
    for e in range(NEXP):
        csel[e, e, :] = 1.0
        csel[16 + e, e, :] = 1.0
    csel = csel.reshape(32, NEXP * 128)
    cident = np.eye(128, dtype=np.float32)
    return cmask, cpool, csel, cident


def host_pvec(norm1_w, norm2_w, pool_scale, b_gate, gla_norm_w, final_norm_w):
    pv = np.zeros((128, PV_COLS), np.float32)
    for l in range(NL):
        o = l * PV_L
        pv[:, o + PV_N1:o + PV_N1 + 8] = np.asarray(norm1_w[l]).reshape(8, 128).T
        pv[:, o + PV_N2:o + PV_N2 + 8] = np.asarray(norm2_w[l]).reshape(8, 128).T
        pv[:, o + PV_PS:o + PV_PS + 8] = np.asarray(pool_scale[l]).reshape(8, 128).T
        pv[:, o + PV_BG:o + PV_BG + 4] = np.asarray(b_gate[l]).reshape(4, 128).T
        pv[:, o + PV_GN:o + PV_GN + 2] = np.asarray(gla_norm_w[l]).reshape(2, 128).T
    pv[:, PV_FN:PV_FN + 8] = np.asarray(final_norm_w).reshape(8, 128).T
    return pv


_NC_CACHE = {}


def make_in_maps(inputs, n_cores=8):
    f = lambda k: np.ascontiguousarray(np.asarray(inputs[k], dtype=np.float32))
    cmask, cpool, csel, cident = host_constants()
    pv = host_pvec(f("norm1_w"), f("norm2_w"), f("pool_scale"), f("b_gate"), f("gla_norm_w"), f("final_norm_w"))
    w_router = np.ascontiguousarray(np.concatenate([f("w_router_group"), f("w_router_expert")], axis=-1))
    shared = {
        "meta": f("meta_tokens"), "w_in": f("w_in"), "w_gate_up": f("w_gate_up"), "w_pool_grp": f("w_pool_grp"),
        "w_br_gla": f("w_br_gla"), "w_br_pool": f("w_br_pool"), "w_out": f("w_out"), "w_router": w_router,
        "w_exp_gate": f("w_exp_gate"), "w_exp_up": f("w_exp_up"), "w_exp_down": f("w_exp_down"),
        "pvec": pv, "cmask": cmask, "cpool": cpool, "csel": csel, "cident": cident,
    }
    x = f("x")
    maps = []
    for c in range(n_cores):
        m = dict(shared)
        m["x"] = np.ascontiguousarray(x[2 * c:2 * c + 2])
        maps.append(m)
    return maps


def kernel(**inputs):
    if "nc" not in _NC_CACHE:
        _NC_CACHE["nc"] = build_program()
    nc = _NC_CACHE["nc"]
    maps = make_in_maps(inputs, 8)
    res = run_bass_kernel_spmd(nc, maps, core_ids=list(range(8)))
    outs = [np.asarray(r["out"]) for r in res.results]
    return np.concatenate(outs, axis=0).astype(np.float32)
```

```python
import contextlib
import numpy as np
import concourse.bass as bass
import concourse.mybir as mybir
from concourse.bass_utils import run_bass_kernel_spmd

F32 = mybir.dt.float32
BF16 = mybir.dt.bfloat16
U8 = mybir.dt.uint8
AF = mybir.ActivationFunctionType
ALU = mybir.AluOpType

ENGS = ["pe", "act", "dve", "pool", "sp"]

D = 1024
NL = 2
SEQ = 2048
NMETA = 16
NT_SEQ = 17
GROUPS = [(0, 9), (9, 8)]
TMAX = 9 * 128
IN_COLS = 6160
C_Q, C_K, C_V, C_OG, C_GL, C_U, C_G0, C_G1 = 0, 512, 1024, 2048, 3072, 3088, 4112, 5136
EPS = 1e-6
QSCALE = 128.0 ** -0.5
NEXP = 16

PV_N1, PV_N2, PV_PS, PV_BG, PV_GN = 0, 8, 16, 24, 28
PV_L = 30
PV_FN = 2 * PV_L
PV_COLS = PV_FN + 8


class Op:
    __slots__ = ("eng", "fn", "deps", "dmadeps", "idx", "group", "milestone", "count", "name")


class Prog:
    def __init__(self, nc):
        self.nc = nc
        self.ops = {e: [] for e in ENGS}
        self.lastw = {}
        self.readers = {}
        self.dma_total = {}
        self.stack = contextlib.ExitStack()

    def sb(self, name, shape, dt):
        return self.stack.enter_context(self.nc.sbuf_tensor(name, list(shape), dt))

    def ps(self, name, shape, dt=F32):
        return self.stack.enter_context(self.nc.psum_tensor(name, list(shape), dt))

    def add(self, eng, fn, reads=(), writes=(), group=None, name=""):
        op = Op()
        op.eng = eng
        op.fn = fn
        op.group = group
        op.name = name or getattr(self, 'phase', '')
        op.milestone = False
        op.count = 0
        op.idx = len(self.ops[eng])
        deps = set()
        dmadeps = {}

        def dep_on(o):
            if o.group is not None:
                g = o.group
                dmadeps[g] = max(dmadeps.get(g, 0), self.dma_total[g])
            else:
                deps.add(o)

        for k in reads:
            o = self.lastw.get(k)
            if o is not None:
                dep_on(o)
        for k in writes:
            o = self.lastw.get(k)
            if o is not None:
                dep_on(o)
            for r in self.readers.get(k, ()):
                if r.eng == eng and eng == "pe" and group is None:
                    continue
                dep_on(r)
        if group is not None:
            self.dma_total[group] = self.dma_total.get(group, 0) + 1
        fdeps = set()
        for o in deps:
            if o.eng == "pe" and eng == "pe" and group is None:
                continue
            fdeps.add(o)
        op.deps = fdeps
        op.dmadeps = dmadeps
        for k in reads:
            self.readers.setdefault(k, []).append(op)
        for k in writes:
            self.lastw[k] = op
            self.readers[k] = []
        self.ops[eng].append(op)
        return op

    def emit(self):
        nc = self.nc
        for e in ENGS:
            for op in self.ops[e]:
                for d in op.deps:
                    d.milestone = True
        for e in ENGS:
            c = 0
            for op in self.ops[e]:
                if op.group is None and op.milestone:
                    c += 1
                    op.count = c
        st = self.stack
        sems = {e: st.enter_context(nc.semaphore("s_" + e)) for e in ENGS}
        gsems = {g: st.enter_context(nc.semaphore("g_%s" % (g,))) for g in self.dma_total}
        block = st.enter_context(nc.Block())
        ops = self.ops

        def run(e, engobj):
            waited = {}
            for op in ops[e]:
                need = {}
                for d in op.deps:
                    key = ("e", d.eng)
                    need[key] = max(need.get(key, 0), d.count)
                for g, tot in op.dmadeps.items():
                    need[("g", g)] = max(need.get(("g", g), 0), tot * 16)
                for key, val in need.items():
                    if waited.get(key, 0) >= val:
                        continue
                    waited[key] = val
                    sem = sems[key[1]] if key[0] == "e" else gsems[key[1]]
                    engobj.wait_ge(sem, val)
                inst = op.fn(engobj)
                op.fn = inst
                if op.group is not None:
                    inst.then_inc(gsems[op.group], 16)
                elif op.milestone:
                    inst.then_inc(sems[e], 1)
            if e == "sp":
                for g, tot in self.dma_total.items():
                    engobj.wait_ge(gsems[g], tot * 16)

        @block.tensor
        def _(eng):
            run("pe", eng)

        @block.scalar
        def _(eng):
            run("act", eng)

        @block.vector
        def _(eng):
            run("dve", eng)

        @block.gpsimd
        def _(eng):
            run("pool", eng)

        @block.sync
        def _(eng):
            run("sp", eng)

    def close(self):
        self.stack.close()


def MM(out, lhsT, rhs, start, stop):
    return lambda e: e.matmul(out, lhsT=lhsT, rhs=rhs, start=start, stop=stop)


def TR(out, in_, identity):
    return lambda e: e.transpose(out=out, in_=in_, identity=identity)


def ACT(out, in_, func, **kw):
    return lambda e: e.activation(out=out, in_=in_, func=func, **kw)


def TT(out, in0, in1, op):
    return lambda e: e.tensor_tensor(out=out, in0=in0, in1=in1, op=op)


def STT(out, in0, scalar, in1, op0, op1):
    return lambda e: e.scalar_tensor_tensor(out=out, in0=in0, scalar=scalar, in1=in1, op0=op0, op1=op1)


def TS(out, in0, scalar1, op0):
    return lambda e: e.tensor_scalar(out=out, in0=in0, scalar1=scalar1, scalar2=None, op0=op0)


def TRED(out, in_, op):
    return lambda e: e.tensor_reduce(out=out, in_=in_, axis=mybir.AxisListType.X, op=op)


def CPY(out, in_):
    return lambda e: e.tensor_copy(out=out, in_=in_)


def MSET(ap, v):
    return lambda e: e.memset(ap, v)


def DMA(out, in_):
    return lambda e: e.dma_start(out=out, in_=in_)


def SCAN(out, data0, data1):
    return lambda e: e.tensor_tensor_scan(out=out, data0=data0, data1=data1, initial=0.0, op0=ALU.mult, op1=ALU.add)


def CPRED(out, mask, data):
    return lambda e: e.copy_predicated(out=out, mask=mask, data=data)


def RECIP(out, in_):
    return lambda e: e.reciprocal(out=out, in_=in_)


def I(name, *args, **kw):
    return lambda e: getattr(e, name)(*args, **kw)


def ntiles_of(ntl):
    out = []
    t = 0
    while t < ntl:
        n = min(4, ntl - t)
        out.append((t, n))
        t += n
    return out


def build_program(debug=None, n_seq=2, n_layers=NL, groups=GROUPS, phases=('n1', 'gla', 'br0', 'pool', 'br1', 'n2', 'route', 'moe')):
    nc = bass.Bass("TRN2", target_bir_lowering=False)

    def din(name, shape, dt=F32):
        return nc.dram_tensor(name, list(shape), dt, kind="ExternalInput").ap()

    x = din("x", [2, SEQ, D])
    meta = din("meta", [NMETA, D])
    w_in = din("w_in", [NL, D, IN_COLS])
    w_gu = din("w_gate_up", [NL, 16, 512])
    w_pool = din("w_pool_grp", [NL, 4, 256, 256])
    w_brg = din("w_br_gla", [NL, D, D])
    w_brp = din("w_br_pool", [NL, D, D])
    w_out = din("w_out", [NL, D, D])
    w_rt = din("w_router", [NL, D, 20])
    w_eg = din("w_exp_gate", [NL, NEXP, D, 512])
    w_eu = din("w_exp_up", [NL, NEXP, D, 512])
    w_ed = din("w_exp_down", [NL, NEXP, 512, D])
    pvec_d = din("pvec", [128, PV_COLS])
    cmask_d = din("cmask", [2, 128, 128], U8)
    cpool_d = din("cpool", [12, 128, 128])
    csel_d = din("csel", [32, NEXP * 128])
    cident_d = din("cident", [128, 128])
    out = nc.dram_tensor("out", [2, SEQ, D], F32, kind="ExternalOutput").ap()
    dbg = None
    if debug:
        dbg = nc.dram_tensor("dbg", [128, 8, TMAX], F32, kind="ExternalOutput").ap()

    P = Prog(nc)
    hT = P.sb("hT", [128, 8, TMAX], F32)
    hnT = P.sb("hnT", [128, 8, TMAX], BF16)
    yg = P.sb("yg", [128, 8, TMAX], BF16)
    NSLOT = 6
    wsl = [P.sb("wsl%d" % i, [128, 4096], BF16) for i in range(NSLOT)]
    ft = [P.sb("ft%d" % i, [128, 512], F32) for i in range(6)]
    bt = [P.sb("bt%d" % i, [128, 512], BF16) for i in range(12)]
    xin = P.sb("xin", [128, D], F32)
    ost = xin
    utok = P.sb("utok", [128, 5, D], BF16)
    uprev = [P.sb("uprev%d" % l, [128, D], BF16) for l in range(NL)]
    S32 = [P.sb("S32_%d" % l, [128, 4, 256], F32) for l in range(NL)]
    Sbf = [P.sb("Sbf_%d" % l, [128, 4, 256], BF16) for l in range(NL)]
    vtok = P.sb("vtok", [128, 8, 256], BF16)
    attn = [P.sb("attn%d" % i, [128, 128], BF16) for i in range(4)]
    kitok = [P.sb("kitok%d" % i, [128, 128], BF16) for i in range(4)]
    SbfT = P.sb("SbfT", [128, 3, 256], BF16)
    wglow = P.sb("wglow", [128, 8, 16], BF16)
    wgu_sb = P.sb("wgu_sb", [16, 512], BF16)
    wpool_sb = P.sb("wpool_sb", [128, 4, 2, 256], BF16)
    wrt_sb = P.sb("wrt_sb", [128, 8, 20], F32)
    pvec = P.sb("pvec_sb", [128, PV_COLS], F32)
    negb = P.sb("negb", [128, NL * 4], F32)
    ident_f = P.sb("ident_f", [128, 128], F32)
    ident_b = P.sb("ident_b", [128, 128], BF16)
    ones_b = P.sb("ones_b", [128, 128], BF16)
    ones_f = P.sb("ones_f", [128, 128], F32)
    cmask = P.sb("cmask_sb", [128, 2, 128], U8)
    cpool = P.sb("cpool_sb", [128, 12, 128], BF16)
    csel = P.sb("csel_sb", [32, NEXP * 128], BF16)
    combT = P.sb("combT", [32, TMAX], BF16)
    glowT = combT[0:16, :]
    RW = 32
    rt = [P.sb("rt%d" % i, [128, 9, RW if i == 7 else 20], F32) for i in range(8)]
    pb = [P.ps("pb%d" % i, [128, 512], F32) for i in range(7)]
    pbt = P.ps("pbt", [128, 1024], BF16)

    def tk(name, c, t0, n=1):
        return [(name, c, t) for t in range(t0, t0 + n)]

    def tk8(name, t0, n=1):
        return [(name, c, t) for c in range(8) for t in range(t0, t0 + n)]

    P.add("sp", I("dma_start", out=pvec[:], in_=pvec_d), writes=["pvec"], group="c0")
    P.add("sp", I("dma_start", out=ident_f[:], in_=cident_d), writes=["ident_f"], group="c0")
    P.add("sp", I("dma_start", out=cmask[:], in_=cmask_d.rearrange("a p n -> p a n")), writes=["cmask"], group="c0")
    P.add("pool", I("dma_start", out=ident_b[:], in_=cident_d), writes=["ident_b"], group="c1")
    P.add("pool", I("dma_start", out=cpool[:], in_=cpool_d.rearrange("a p n -> p a n")), writes=["cpool"], group="c1")
    P.add("pool", I("dma_start", out=csel[:], in_=csel_d), writes=["csel"], group="c1")
    P.add("dve", I("memset", ones_b[:], 1.0), writes=["ones_b"])
    P.add("dve", I("memset", ones_f[:], 1.0), writes=["ones_f"])
    for l in range(NL):
        P.add("dve", I("tensor_scalar", out=negb[:, l * 4:(l + 1) * 4], in0=pvec[:, l * PV_L + PV_BG:l * PV_L + PV_BG + 4],
                                                    scalar1=-1.0, scalar2=None, op0=ALU.mult),
              reads=["pvec"], writes=[("negb", l)])

    slot_rr = [0]

    def wload(dram_ap, shape3, nm):
        s = slot_rr[0] % NSLOT
        slot_rr[0] += 1
        a, b = shape3
        view = wsl[s][:, 0:a * b].rearrange("p (a b) -> p a b", a=a)
        key = ("wsl", s)
        P.add("pool", I("dma_start", out=view, in_=dram_ap), writes=[key], group="w%d" % s, name=nm)
        return view, key

    def w_in_cols(l, c0, n):
        return w_in[l].rearrange("(kc p) n -> p kc n", p=128)[:, :, c0:c0 + n]

    def w_sq(wd, l, half):
        return wd[l].rearrange("(kc p) n -> p kc n", p=128)[:, :, half * 512:(half + 1) * 512]

    def norm_ntile(l_col, a, n, t0, ntl, dst_fn, dst_keys_fn):
        for c in range(8):
            P.add("act", I("activation", out=bt[c][:, 0:n], in_=hT[:, c, a:a + n], func=AF.Square),
                  reads=tk("h", c, t0, ntl), writes=[("bt", c)])
        for c in range(8):
            P.add("pe", I("matmul", pb[2][:, 0:n], lhsT=ones_b[:], rhs=bt[c][:, 0:n], start=(c == 0), stop=(c == 7)),
                  reads=[("bt", c), "ones_b"], writes=[("pb", 2)])
        P.add("act", I("activation", out=ft[0][:, 0:n], in_=pb[2][:, 0:n], func=AF.Ln, scale=1.0 / D, bias=EPS),
              reads=[("pb", 2)], writes=[("ft", 0)])
        P.add("act", I("activation", out=ft[1][:, 0:n], in_=ft[0][:, 0:n], func=AF.Exp, scale=-0.5),
              reads=[("ft", 0)], writes=[("ft", 1)])
        for c in range(8):
            P.add("dve", I("scalar_tensor_tensor", out=dst_fn(c), in0=hT[:, c, a:a + n], scalar=pvec[:, l_col + c:l_col + c + 1],
                                                               in1=ft[1][:, 0:n], op0=ALU.mult, op1=ALU.mult),
                  reads=tk("h", c, t0, ntl) + [("ft", 1), "pvec"], writes=dst_keys_fn(c))

    for seq in range(n_seq):
        for gi, (g0, gn) in enumerate(groups):
            T = gn * 128
            NTS = ntiles_of(gn)
            c_lo = 112 if g0 == 0 else 0
            c_tot = T - c_lo
            npart = (c_tot + 511) // 512
            CNT = []
            ca = c_lo
            for pi in range(npart):
                cn = c_tot // npart + (1 if pi < c_tot % npart else 0)
                CNT.append((ca, cn, ca // 128, (ca + cn - 1) // 128 - ca // 128 + 1))
                ca += cn
            P.phase = 'load'
            for tl in range(gn):
                gt = g0 + tl
                alt = (tl % 2 == 1)
                if gt == 0:
                    P.add("dve", I("memset", xin[:], 0.0), writes=["xin"])
                    P.add("sp", I("dma_start", out=xin[112:128, :], in_=meta), writes=["xin"], group="xin")
                elif not alt:
                    P.add("sp", I("dma_start", out=xin[:], in_=x[seq, (gt - 1) * 128:gt * 128, :]), writes=["xin"], group="xin")
                else:
                    for hh in range(2):
                        P.add("sp", I("dma_start", out=ft[hh][:], in_=x[seq, (gt - 1) * 128:gt * 128, hh * 512:(hh + 1) * 512]), writes=[("ft", hh)], group="xin2")
                for c in range(8):
                    b = c // 4
                    if alt and gt != 0:
                        src_ap, src_k = ft[c // 4][:, (c % 4) * 128:(c % 4 + 1) * 128], ("ft", c // 4)
                    else:
                        src_ap, src_k = xin[:, c * 128:(c + 1) * 128], "xin"
                    P.add("pe", I("transpose", out=pb[b][:, (c % 4) * 128:(c % 4 + 1) * 128], in_=src_ap, identity=ident_f[:]),
                          reads=[src_k, "ident_f"], writes=[("pb", b)])
                for b in range(2):
                    P.add("act", I("activation", out=hT[:, b * 4:(b + 1) * 4, tl * 128:(tl + 1) * 128],
                                                                   in_=pb[b][:].rearrange("p (c t) -> p c t", c=4), func=AF.Copy),
                          reads=[("pb", b)], writes=[("h", c, tl) for c in range(b * 4, b * 4 + 4)])
            if g0 == 0:
                for l in range(NL):
                    P.add("pool", I("memset", S32[l][:], 0.0), writes=[("S32", l, h) for h in range(4)])
                    P.add("pool", I("memset", Sbf[l][:], 0.0), writes=[("Sbf", l, h) for h in range(4)])
                    P.add("pool", I("memset", uprev[l][:], 0.0), writes=[("uprev", l)])
                for i in range(4):
                    P.add("pool", I("memset", attn[i][:], 0.0), writes=[("attn", i)])

            for l in range(n_layers):
                pl = l * PV_L
                P.add("pool", I("dma_start", out=wglow[:], in_=w_in_cols(l, C_GL, 16)), writes=["wglow"], group="ws")
                P.add("pool", I("dma_start", out=wgu_sb[:], in_=w_gu[l]), writes=["wgu"], group="ws")
                P.add("pool", I("dma_start", out=wpool_sb[:], in_=w_pool[l].rearrange("g (kc p) n -> p g kc n", p=128)), writes=["wpool"], group="ws")
                P.add("sp", I("dma_start", out=wrt_sb[:], in_=w_rt[l].rearrange("(kc p) n -> p kc n", p=128)), writes=["wrt"], group="ws2")

                P.phase = 'norm1'
                for (t0, ntl) in NTS:
                    a, n = t0 * 128, ntl * 128
                    norm_ntile(pl + PV_N1, a, n, t0, ntl, lambda c, a=a, n=n: hnT[:, c, a:a + n], lambda c, t0=t0, ntl=ntl: tk("hn", c, t0, ntl))

                for (t0, ntl) in NTS:
                    a, n = t0 * 128, ntl * 128
                    for kc in range(8):
                        P.add("pe", I("matmul", pb[2][0:16, 0:n], lhsT=wglow[:, kc, :], rhs=hnT[:, kc, a:a + n], start=(kc == 0), stop=(kc == 7)),
                              reads=["wglow"] + tk("hn", kc, t0, ntl), writes=[("pb", 2)])
                    P.add("act", I("activation", out=glowT[:, a:a + n], in_=pb[2][0:16, 0:n], func=AF.Copy),
                          reads=[("pb", 2)], writes=tk("combT", 0, t0, ntl))

                P.phase = 'gla'
                items = [(hd, ni) for hd in (range(4) if 'gla' in phases else []) for ni in range(len(NTS))]
                hw = {}

                def head_weights(hd):
                    if hd in hw:
                        return hw[hd]
                    s = slot_rr[0] % NSLOT
                    slot_rr[0] += 1
                    va = wsl[s][:, 0:4096].rearrange("p (a b) -> p a b", a=8)
                    key = ("wsl", s)
                    for (c0, ncol, off) in ((C_Q + hd * 128, 128, 0), (C_K + hd * 128, 128, 128), (C_V + hd * 256, 256, 256)):
                        P.add("pool", I("dma_start", out=va[:, :, off:off + ncol], in_=w_in_cols(l, c0, ncol)), writes=[key], group="w%d" % s, name="wqkv")
                    wog, kog = wload(w_in_cols(l, C_OG + hd * 256, 256), (8, 256), "wog")
                    hw[hd] = (va, key, wog, kog)
                    return hw[hd]

                def gla_p1(i):
                    hd, ni = items[i]
                    par = i % 2
                    t0, ntl = NTS[ni]
                    a, n = t0 * 128, ntl * 128
                    va, kva, wog, kog = head_weights(hd)
                    B = [bt[0], bt[1], bt[2], bt[3]] if par == 0 else [bt[8], bt[9], bt[10], bt[11]]
                    Bk = [("bt", 0), ("bt", 1), ("bt", 2), ("bt", 3)] if par == 0 else [("bt", 8), ("bt", 9), ("bt", 10), ("bt", 11)]
                    eB, keB = ft[2 + par], ("ft", 2 + par)
                    hnk = lambda kc: tk("hn", kc, t0, ntl)
                    P.phase = 'gla.chain'
                    P.add("pe", I("matmul", pb[2][:, 0:n], lhsT=wgu_sb[:, hd * 128:(hd + 1) * 128], rhs=glowT[:, a:a + n], start=True, stop=True),
                          reads=["wgu"] + tk("combT", 0, t0, ntl), writes=[("pb", 2)])
                    P.add("act", I("activation", out=ft[0][:, 0:n], in_=pb[2][:, 0:n], func=AF.Exp, scale=-1.0, bias=negb[:, l * 4 + hd:l * 4 + hd + 1]),
                          reads=[("pb", 2), ("negb", l)], writes=[("ft", 0)])
                    P.add("act", I("activation", out=ft[0][:, 0:n], in_=ft[0][:, 0:n], func=AF.Ln, bias=1.0),
                          reads=[("ft", 0)], writes=[("ft", 0)])
                    if g0 + t0 == 0:
                        P.add("dve", I("memset", ft[0][:, 0:112], 0.0), reads=[("ft", 0)], writes=[("ft", 0)])
                    for j in range(ntl):
                        P.add("dve", I("tensor_tensor_scan", out=ft[1][:, j * 128:(j + 1) * 128], data0=ones_f[:], data1=ft[0][:, j * 128:(j + 1) * 128],
                                       initial=0.0, op0=ALU.mult, op1=ALU.add),
                              reads=[("ft", 0), "ones_f"], writes=[("ft", 1)])
                    P.add("act", I("activation", out=eB[:, 0:n], in_=ft[1][:, 0:n], func=AF.Exp, scale=-1.0 / 16.0),
                          reads=[("ft", 1)], writes=[keB])
                    P.add("act", I("activation", out=ft[0][:, 0:n], in_=ft[1][:, 0:n], func=AF.Exp, scale=1.0 / 16.0),
                          reads=[("ft", 1)], writes=[("ft", 0)])
                    P.phase = 'gla.qk'
                    for kc in range(8):
                        P.add("pe", I("matmul", pb[0][:, 0:n], lhsT=va[:, kc, 0:128], rhs=hnT[:, kc, a:a + n], start=(kc == 0), stop=(kc == 7)),
                              reads=[kva] + hnk(kc), writes=[("pb", 0)])
                    for kc in range(8):
                        P.add("pe", I("matmul", pb[1][:, 0:n], lhsT=va[:, kc, 128:256], rhs=hnT[:, kc, a:a + n], start=(kc == 0), stop=(kc == 7)),
                              reads=[kva] + hnk(kc), writes=[("pb", 1)])
                    P.add("dve", I("scalar_tensor_tensor", out=B[0][:, 0:n], in0=pb[0][:, 0:n], scalar=QSCALE, in1=eB[:, 0:n], op0=ALU.mult, op1=ALU.mult),
                          reads=[("pb", 0), keB], writes=[Bk[0]])
                    P.add("dve", I("scalar_tensor_tensor", out=B[1][:, 0:n], in0=pb[0][:, 0:n], scalar=QSCALE, in1=ft[0][:, 0:n], op0=ALU.mult, op1=ALU.mult),
                          reads=[("pb", 0), ("ft", 0)], writes=[Bk[1]])
                    P.add("dve", I("tensor_tensor", out=B[2][:, 0:n], in0=pb[1][:, 0:n], in1=ft[0][:, 0:n], op=ALU.mult),
                          reads=[("pb", 1), ("ft", 0)], writes=[Bk[2]])
                    P.add("dve", I("tensor_tensor", out=B[3][:, 0:n], in0=pb[1][:, 0:n], in1=eB[:, 0:n], op=ALU.mult),
                          reads=[("pb", 1), keB], writes=[Bk[3]])
                    P.phase = 'gla.v'
                    for j in range(ntl):
                        jj = j % 2
                        for kc in range(8):
                            P.add("pe", I("matmul", pb[3 + jj][:, 0:256], lhsT=hnT[:, kc, a + j * 128:a + (j + 1) * 128], rhs=va[:, kc, 256:512],
                                          start=(kc == 0), stop=(kc == 7)),
                                  reads=[kva] + tk("hn", kc, t0 + j), writes=[("pb", 3 + jj)])
                        P.add("act", I("activation", out=vtok[:, par * 4 + j, :], in_=pb[3 + jj][:, 0:256], func=AF.Copy),
                              reads=[("pb", 3 + jj)], writes=[("vtok", par * 4 + j)])

                def gla_s(i):
                    hd, ni = items[i]
                    par = i % 2
                    t0, ntl = NTS[ni]
                    a, n = t0 * 128, ntl * 128
                    va, kva, wog, kog = head_weights(hd)
                    B = [bt[0], bt[1], bt[2], bt[3]] if par == 0 else [bt[8], bt[9], bt[10], bt[11]]
                    Bk = [("bt", 0), ("bt", 1), ("bt", 2), ("bt", 3)] if par == 0 else [("bt", 8), ("bt", 9), ("bt", 10), ("bt", 11)]
                    eB, keB = ft[2 + par], ("ft", 2 + par)
                    hnk = lambda kc: tk("hn", kc, t0, ntl)
                    def og_c(c):
                        P.phase = 'gla.og'
                        for kc in range(8):
                            P.add("pe", I("matmul", pb[c][:, 0:n], lhsT=wog[:, kc, c * 128:(c + 1) * 128], rhs=hnT[:, kc, a:a + n], start=(kc == 0), stop=(kc == 7)),
                                  reads=[kog] + hnk(kc), writes=[("pb", c)])
                        sg = ft[4 + c]
                        P.add("act", I("activation", out=sg[:, 0:n], in_=pb[c][:, 0:n], func=AF.Exp, scale=-1.0),
                              reads=[("pb", c)], writes=[("ft", 4 + c)])
                        P.add("act", I("activation", out=sg[:, 0:n], in_=sg[:, 0:n], func=AF.Ln, bias=1.0),
                              reads=[("ft", 4 + c)], writes=[("ft", 4 + c)])
                        P.add("act", I("activation", out=sg[:, 0:n], in_=sg[:, 0:n], func=AF.Exp, scale=-1.0),
                              reads=[("ft", 4 + c)], writes=[("ft", 4 + c)])
                        P.add("dve", I("tensor_tensor", out=bt[4 + c][:, 0:n], in0=pb[c][:, 0:n], in1=sg[:, 0:n], op=ALU.mult),
                              reads=[("pb", c), ("ft", 4 + c)], writes=[("bt", 4 + c)])

                    P.phase = 'gla.scan'
                    for j in range(ntl):
                        P.add("pe", I("transpose", out=pbt[:, j * 128:(j + 1) * 128], in_=B[2][:, j * 128:(j + 1) * 128], identity=ident_b[:]),
                              reads=[Bk[2], "ident_b"], writes=[("pbt", 0)])
                    for j in range(ntl):
                        P.add("act", I("activation", out=kitok[j][:], in_=pbt[:, j * 128:(j + 1) * 128], func=AF.Copy),
                              reads=[("pbt", 0)], writes=[("kitok", j)])
                    P.phase = 'gla.scan'
                    for j in range(ntl):
                        vj = par * 4 + j
                        ja, jb = j * 128, (j + 1) * 128
                        bA = 3 + (j % 2)
                        pA = pb[bA]
                        P.add("pe", I("matmul", pA[:, 0:128], lhsT=B[2][:, ja:jb], rhs=B[0][:, ja:jb], start=True, stop=True),
                              reads=[Bk[2], Bk[0]], writes=[("pb", bA)])
                        P.add("pe", I("matmul", pA[:, 128:256], lhsT=B[3][:, ja:jb], rhs=B[1][:, ja:jb], start=True, stop=True),
                              reads=[Bk[3], Bk[1]], writes=[("pb", bA)])
                        P.add("dve", I("copy_predicated", out=attn[j][:], mask=cmask[:, 0, :], data=pA[:, 0:128]),
                              reads=[("pb", bA), "cmask"], writes=[("attn", j)])
                        P.add("dve", I("copy_predicated", out=attn[j][:], mask=cmask[:, 1, :], data=pA[:, 128:256]),
                              reads=[("pb", bA), "cmask"], writes=[("attn", j)])
                        if j == min(1, ntl - 1):
                            og_c(0)
                            P.phase = 'gla.scan'
                    og_c(1)
                    P.phase = 'gla.scan'
                    for j in range(ntl):
                        vj = par * 4 + j
                        ja, jb = j * 128, (j + 1) * 128
                        bA = 3 + (j % 2)
                        pA = pb[bA]
                        P.add("pe", I("matmul", pA[:, 256:512], lhsT=kitok[j][:], rhs=vtok[:, vj, :], start=True, stop=True),
                              reads=[("kitok", j), ("vtok", vj)], writes=[("pb", bA)])
                        P.add("act", I("activation", out=ft[4 + j // 2][:, (j % 2) * 256:(j % 2) * 256 + 256], in_=pA[:, 256:512], func=AF.Identity, scale=eB[:, jb - 1:jb]),
                              reads=[("pb", bA), keB], writes=[("ft", 4 + j // 2)])
                    P.phase = 'gla.seq'
                    for j in range(ntl):
                        vj = par * 4 + j
                        ja, jb = j * 128, (j + 1) * 128
                        if j == 0:
                            Sprev, kSprev = Sbf[l][:, hd, :], ("Sbf", l, hd)
                        else:
                            Sprev, kSprev = SbfT[:, j - 1, :], ("SbfT", j - 1)
                        for c in range(2):
                            P.add("pe", I("matmul", pb[5 + c][:, ja:jb], lhsT=vtok[:, vj, c * 128:(c + 1) * 128], rhs=attn[j][:], start=True, stop=False),
                                  reads=[("vtok", vj), ("attn", j)], writes=[("pb", 5 + c)])
                            P.add("pe", I("matmul", pb[5 + c][:, ja:jb], lhsT=Sprev[:, c * 128:(c + 1) * 128], rhs=B[0][:, ja:jb], start=False, stop=True),
                                  reads=[kSprev, Bk[0]], writes=[("pb", 5 + c)])
                        if j == ntl - 1:
                            Snext, kSnext = Sbf[l][:, hd, :], ("Sbf", l, hd)
                        else:
                            Snext, kSnext = SbfT[:, j, :], ("SbfT", j)
                        P.add("dve", I("scalar_tensor_tensor", out=Snext, in0=S32[l][:, hd, :], scalar=eB[:, jb - 1:jb],
                                       in1=ft[4 + j // 2][:, (j % 2) * 256:(j % 2) * 256 + 256], op0=ALU.mult, op1=ALU.add),
                              reads=[("S32", l, hd), keB, ("ft", 4 + j // 2)], writes=[kSnext])
                        P.add("dve", I("scalar_tensor_tensor", out=S32[l][:, hd, :], in0=S32[l][:, hd, :], scalar=eB[:, jb - 1:jb],
                                       in1=ft[4 + j // 2][:, (j % 2) * 256:(j % 2) * 256 + 256], op0=ALU.mult, op1=ALU.add),
                              reads=[("S32", l, hd), keB, ("ft", 4 + j // 2)], writes=[("S32", l, hd)])
                    P.phase = 'gla.fin'
                    for c in range(2):
                        P.add("act", I("activation", out=bt[6 + c][:, 0:n], in_=pb[5 + c][:, 0:n], func=AF.Square),
                              reads=[("pb", 5 + c)], writes=[("bt", 6 + c)])
                    for c in range(2):
                        P.add("pe", I("matmul", pb[4][:, 0:n], lhsT=ones_b[:], rhs=bt[6 + c][:, 0:n], start=(c == 0), stop=(c == 1)),
                              reads=[("bt", 6 + c), "ones_b"], writes=[("pb", 4)])
                    P.add("act", I("activation", out=ft[4][:, 0:n], in_=pb[4][:, 0:n], func=AF.Ln, scale=1.0 / 256.0, bias=EPS),
                          reads=[("pb", 4)], writes=[("ft", 4)])
                    P.add("act", I("activation", out=ft[4][:, 0:n], in_=ft[4][:, 0:n], func=AF.Exp, scale=-0.5),
                          reads=[("ft", 4)], writes=[("ft", 4)])
                    for c in range(2):
                        P.add("dve", I("scalar_tensor_tensor", out=ft[5][:, 0:n], in0=pb[5 + c][:, 0:n], scalar=pvec[:, pl + PV_GN + c:pl + PV_GN + c + 1],
                                       in1=ft[4][:, 0:n], op0=ALU.mult, op1=ALU.mult),
                              reads=[("pb", 5 + c), ("ft", 4), "pvec"], writes=[("ft", 5)])
                        P.add("dve", I("tensor_tensor", out=yg[:, hd * 2 + c, a:a + n], in0=ft[5][:, 0:n], in1=bt[4 + c][:, 0:n], op=ALU.mult),
                              reads=[("ft", 5), ("bt", 4 + c)], writes=tk("yg", hd * 2 + c, t0, ntl))

                if items:
                    gla_p1(0)
                for i in range(len(items)):
                    if i + 1 < len(items):
                        gla_p1(i + 1)
                    gla_s(i)

                P.phase = 'br0'
                def branch(wbr_d, ccol, src, srcname):
                    wb, wg = [], []
                    for hf in range(2):
                        wb.append(wload(w_sq(wbr_d, l, hf), (8, 512), "wbr"))
                        wg.append(wload(w_in_cols(l, ccol + hf * 512, 512), (8, 512), "wgate"))
                    wo = [wload(w_sq(w_out, l, hf), (8, 512), "wout") for hf in range(2)]
                    for (a, n, t0, ntl) in CNT:
                        for m in range(8):
                            hf, mo = m // 4, (m % 4) * 128
                            pbr, pgt = pb[(m % 2) * 2], pb[(m % 2) * 2 + 1]
                            for kc in range(8):
                                P.add("pe", I("matmul", pbr[:, 0:n], lhsT=wb[hf][0][:, kc, mo:mo + 128], rhs=src[:, kc, a:a + n], start=(kc == 0), stop=(kc == 7)),
                                      reads=[wb[hf][1]] + tk(srcname, kc, t0, ntl), writes=[("pb", (m % 2) * 2)])
                            for kc in range(8):
                                P.add("pe", I("matmul", pgt[:, 0:n], lhsT=wg[hf][0][:, kc, mo:mo + 128], rhs=hnT[:, kc, a:a + n], start=(kc == 0), stop=(kc == 7)),
                                      reads=[wg[hf][1]] + tk("hn", kc, t0, ntl), writes=[("pb", (m % 2) * 2 + 1)])
                            P.add("act", I("activation", out=ft[m % 2][:, 0:n], in_=pgt[:, 0:n], func=AF.Sigmoid),
                                  reads=[("pb", (m % 2) * 2 + 1)], writes=[("ft", m % 2)])
                            P.add("dve", I("tensor_tensor", out=bt[m][:, 0:n], in0=pbr[:, 0:n], in1=ft[m % 2][:, 0:n], op=ALU.mult),
                                  reads=[("pb", (m % 2) * 2), ("ft", m % 2)], writes=[("bt", m)])
                        for m in range(8):
                            hf, mo = m // 4, (m % 4) * 128
                            po = pb[4 + m % 2]
                            for kc in range(8):
                                P.add("pe", I("matmul", po[:, 0:n], lhsT=wo[hf][0][:, kc, mo:mo + 128], rhs=bt[kc][:, 0:n], start=(kc == 0), stop=(kc == 7)),
                                      reads=[wo[hf][1], ("bt", kc)], writes=[("pb", 4 + m % 2)])
                            P.add("dve", I("tensor_tensor", out=hT[:, m, a:a + n], in0=po[:, 0:n], in1=hT[:, m, a:a + n], op=ALU.add),
                                  reads=[("pb", 4 + m % 2)] + tk("h", m, t0, ntl), writes=tk("h", m, t0, ntl))

                if 'br0' in phases:
                    branch(w_brg, C_G0, yg, "yg")

                P.phase = 'pool'
                wu = [wload(w_in_cols(l, C_U + hf * 512, 512), (8, 512), "wu") for hf in range(2)]
                P.add("pool", I("tensor_copy", out=utok[:, 0, :], in_=uprev[l][:]), reads=[("uprev", l)], writes=[("utok", 0)])
                for (t0, ntl) in NTS:
                    a, n = t0 * 128, ntl * 128
                    for j in range(ntl):
                        for hf in range(2):
                            pu = pb[hf]
                            for kc in range(8):
                                P.add("pe", I("matmul", pu[:, 0:512], lhsT=hnT[:, kc, a + j * 128:a + (j + 1) * 128], rhs=wu[hf][0][:, kc, :], start=(kc == 0), stop=(kc == 7)),
                                      reads=[wu[hf][1]] + tk("hn", kc, t0 + j), writes=[("pb", hf)])
                            P.add("act", I("activation", out=utok[:, j + 1, hf * 512:(hf + 1) * 512], in_=pu[:, 0:512], func=AF.Copy),
                                  reads=[("pb", hf)], writes=[("utok", j + 1, hf)])
                    for g in range(4):
                        for cc in range(2):
                            c = g * 2 + cc
                            pp = pb[2 + c % 2]
                            for j in range(ntl):
                                first = (g0 + t0 + j == 0)
                                pcur = cpool[:, (8 + g) if first else g, :]
                                P.add("pe", I("matmul", pp[:, j * 128:(j + 1) * 128], lhsT=utok[:, j, c * 128:(c + 1) * 128], rhs=cpool[:, 4 + g, :], start=True, stop=False),
                                      reads=[("utok", j, 0), ("utok", j, 1), ("utok", j), "cpool"], writes=[("pb", 2 + c % 2)])
                                P.add("pe", I("matmul", pp[:, j * 128:(j + 1) * 128], lhsT=utok[:, j + 1, c * 128:(c + 1) * 128], rhs=pcur, start=False, stop=True),
                                      reads=[("utok", j + 1, 0), ("utok", j + 1, 1), ("utok", j + 1), "cpool"], writes=[("pb", 2 + c % 2)])
                            P.add("act", I("activation", out=bt[8 + c % 4][:, 0:n], in_=pp[:, 0:n], func=AF.Copy),
                                  reads=[("pb", 2 + c % 2)], writes=[("bt", 8 + c % 4)])
                        for oc in range(2):
                            pm = pb[4 + oc]
                            for cc in range(2):
                                c = g * 2 + cc
                                P.add("pe", I("matmul", pm[:, 0:n], lhsT=wpool_sb[:, g, cc, oc * 128:(oc + 1) * 128], rhs=bt[8 + c % 4][:, 0:n], start=(cc == 0), stop=(cc == 1)),
                                      reads=["wpool", ("bt", 8 + c % 4)], writes=[("pb", 4 + oc)])
                            mo = g * 2 + oc
                            P.add("act", I("activation", out=yg[:, mo, a:a + n], in_=pm[:, 0:n], func=AF.Identity, scale=pvec[:, pl + PV_PS + mo:pl + PV_PS + mo + 1]),
                                  reads=[("pb", 4 + oc), "pvec"], writes=tk("yg", mo, t0, ntl))
                    P.add("pool", I("tensor_copy", out=utok[:, 0, :], in_=utok[:, ntl, :]),
                          reads=[("utok", ntl, 0), ("utok", ntl, 1), ("utok", ntl)], writes=[("utok", 0)])
                P.add("pool", I("tensor_copy", out=uprev[l][:], in_=utok[:, 0, :]), reads=[("utok", 0)], writes=[("uprev", l)])

                P.phase = 'br1'
                if 'br1' in phases:
                    branch(w_brp, C_G1, yg, "yg")

                if debug == ("mix", l) and seq == 0 and gi == 0:
                    P.add("sp", I("dma_start", out=dbg[:, :, 0:T], in_=hT[:, :, 0:T]), reads=tk8("h", 0, gn), group="dbg")

                P.phase = 'norm2'
                for c in range(8):
                    P.add("dve", I("tensor_scalar", out=wrt_sb[:, c, :], in0=wrt_sb[:, c, :], scalar1=pvec[:, pl + PV_N2 + c:pl + PV_N2 + c + 1], scalar2=None, op0=ALU.mult),
                          reads=["wrt", "pvec"], writes=["wrt"])
                for tl in range(gn):
                    a = tl * 128
                    bi = 3 + tl % 4
                    for c in range(8):
                        P.add("pe", I("matmul", pb[bi][:, 0:20], lhsT=hT[:, c, a:a + 128], rhs=wrt_sb[:, c, :], start=(c == 0), stop=(c == 7)),
                              reads=tk("h", c, tl) + ["wrt"], writes=[("pb", bi)])
                    P.add("act", I("activation", out=rt[0][:, tl, 0:20], in_=pb[bi][:, 0:20], func=AF.Copy),
                          reads=[("pb", bi)], writes=[("rt", 0)])
                for (t0, ntl) in NTS:
                    a, n = t0 * 128, ntl * 128
                    norm_ntile(pl + PV_N2, a, n, t0, ntl, lambda c, a=a, n=n: hnT[:, c, a:a + n], lambda c, t0=t0, ntl=ntl: tk("hn", c, t0, ntl))
                    for j in range(ntl):
                        P.add("pe", I("matmul", pb[1][:, t0 + j:t0 + j + 1], lhsT=ft[1][0:1, j * 128:(j + 1) * 128], rhs=ones_f[0:1, 0:1], start=True, stop=True),
                              reads=[("ft", 1), "ones_f"], writes=[("pb", 1)])
                P.add("act", I("activation", out=rt[6][:, 0:gn, 19], in_=pb[1][:, 0:gn], func=AF.Copy),
                      reads=[("pb", 1)], writes=[("rt", 6)])
                P.add("dve", I("tensor_tensor", out=rt[0][:, 0:gn, 0:20], in0=rt[0][:, 0:gn, 0:20],
                               in1=rt[6][:, 0:gn, 19].unsqueeze(2).broadcast_to([128, gn, 20]), op=ALU.mult),
                      reads=[("rt", 0), ("rt", 6)], writes=[("rt", 0)])

                P.phase = 'route'
                G = gn
                Lg = lambda t: t[:, 0:G, 0:4]
                Le4 = rt[0][:, 0:G, 4:20].rearrange("p g (a b) -> p g a b", a=4)
                Le4T = rt[0][:, 0:G, 4:20].rearrange("p g (a b) -> p g b a", a=4)

                def dv(fn, reads, writes):
                    P.add("dve", fn, reads=[("rt", i) for i in reads], writes=[("rt", i) for i in writes])

                def bc(ap2, k):
                    return ap2.unsqueeze(2).broadcast_to([128, G, k])

                mg = rt[1][:, 0:G, 0]
                dv(I("tensor_reduce", out=mg, in_=Lg(rt[0]), axis=mybir.AxisListType.X, op=ALU.max), [0], [1])
                dv(I("tensor_tensor", out=rt[1][:, 0:G, 4:8], in0=Lg(rt[0]), in1=bc(mg, 4), op=ALU.is_equal), [0, 1], [1])
                dv(I("tensor_tensor", out=rt[2][:, 0:G, 0:4], in0=Lg(rt[0]), in1=bc(mg, 4), op=ALU.subtract), [0, 1], [2])
                P.add("act", I("activation", out=rt[2][:, 0:G, 0:4], in_=rt[2][:, 0:G, 0:4], func=AF.Exp), reads=[("rt", 2)], writes=[("rt", 2)])
                dv(I("tensor_reduce", out=rt[2][:, 0:G, 4], in_=rt[2][:, 0:G, 0:4], axis=mybir.AxisListType.X, op=ALU.add), [2], [2])
                dv(I("tensor_tensor", out=rt[3][:, 0:G, 0:16].rearrange("p g (a b) -> p g a b", a=4), in0=Le4,
                                             in1=rt[1][:, 0:G, 4:8].unsqueeze(3).broadcast_to([128, G, 4, 4]), op=ALU.mult), [0, 1], [3])
                dv(I("tensor_reduce", out=rt[3][:, 0:G, 16:20], in_=rt[3][:, 0:G, 0:16].rearrange("p g (a b) -> p g b a", a=4), axis=mybir.AxisListType.X, op=ALU.add), [3], [3])
                les = lambda: rt[3][:, 0:G, 16:20]
                m1 = rt[4][:, 0:G, 0]
                dv(I("tensor_reduce", out=m1, in_=les(), axis=mybir.AxisListType.X, op=ALU.max), [3], [4])
                dv(I("tensor_tensor", out=rt[4][:, 0:G, 4:8], in0=les(), in1=bc(m1, 4), op=ALU.is_equal), [3, 4], [4])
                dv(I("scalar_tensor_tensor", out=rt[4][:, 0:G, 8:12], in0=rt[4][:, 0:G, 4:8], scalar=-1e30, in1=les(), op0=ALU.mult, op1=ALU.add), [3, 4], [4])
                m2 = rt[4][:, 0:G, 1]
                dv(I("tensor_reduce", out=m2, in_=rt[4][:, 0:G, 8:12], axis=mybir.AxisListType.X, op=ALU.max), [4], [4])
                dv(I("tensor_tensor", out=rt[4][:, 0:G, 12:16], in0=rt[4][:, 0:G, 8:12], in1=bc(m2, 4), op=ALU.is_equal), [4], [4])
                dv(I("tensor_tensor", out=rt[5][:, 0:G, 0], in0=m2, in1=m1, op=ALU.subtract), [4], [5])
                P.add("act", I("activation", out=rt[5][:, 0:G, 0], in_=rt[5][:, 0:G, 0], func=AF.Exp), reads=[("rt", 5)], writes=[("rt", 5)])
                dv(I("scalar_tensor_tensor", out=rt[5][:, 0:G, 1], in0=rt[5][:, 0:G, 0], scalar=1.0, in1=rt[2][:, 0:G, 4], op0=ALU.add, op1=ALU.mult), [5, 2], [5])
                dv(I("reciprocal", out=rt[5][:, 0:G, 2], in_=rt[5][:, 0:G, 1]), [5], [5])
                dv(I("tensor_tensor", out=rt[5][:, 0:G, 3], in0=rt[5][:, 0:G, 2], in1=rt[5][:, 0:G, 0], op=ALU.mult), [5], [5])
                dv(I("tensor_tensor", out=rt[5][:, 0:G, 4:8], in0=rt[4][:, 0:G, 4:8], in1=bc(rt[5][:, 0:G, 2], 4), op=ALU.mult), [4, 5], [5])
                dv(I("tensor_tensor", out=rt[5][:, 0:G, 8:12], in0=rt[4][:, 0:G, 12:16], in1=bc(rt[5][:, 0:G, 3], 4), op=ALU.mult), [4, 5], [5])
                dv(I("tensor_tensor", out=rt[5][:, 0:G, 4:8], in0=rt[5][:, 0:G, 4:8], in1=rt[5][:, 0:G, 8:12], op=ALU.add), [5], [5])
                dv(I("tensor_tensor", out=rt[6][:, 0:G, 0:16].rearrange("p g (a b) -> p g a b", a=4),
                                             in0=rt[1][:, 0:G, 4:8].unsqueeze(3).broadcast_to([128, G, 4, 4]),
                                             in1=rt[5][:, 0:G, 4:8].unsqueeze(2).broadcast_to([128, G, 4, 4]), op=ALU.mult), [1, 5], [6])
                P.add("dve", I("tensor_copy", out=bt[0][:, 0:G * 16].rearrange("p (g k) -> p g k", g=G), in_=rt[6][:, 0:G, 0:16]),
                      reads=[("rt", 6)], writes=[("bt", 0)])
                P.add("dve", I("tensor_copy", out=rt[7][:, 0:G, 0:16], in_=bt[0][:, 0:G * 16].rearrange("p (g k) -> p g k", g=G)),
                      reads=[("bt", 0)], writes=[("rt", 7)])
                dv(I("tensor_tensor", out=rt[7][:, 0:G, 16:32], in0=rt[6][:, 0:G, 0:16], in1=rt[7][:, 0:G, 0:16], op=ALU.subtract), [6, 7], [7])
                for tl in range(gn):
                    P.add("pe", I("transpose", out=pb[3][0:32, (tl % 4) * 128:(tl % 4 + 1) * 128], in_=rt[7][:, tl, 0:32], identity=ident_f[:]),
                          reads=[("rt", 7), "ident_f"], writes=[("pb", 3)])
                    P.add("act", I("activation", out=combT[:, tl * 128:(tl + 1) * 128], in_=pb[3][0:32, (tl % 4) * 128:(tl % 4 + 1) * 128], func=AF.Copy),
                          reads=[("pb", 3)], writes=tk("combT", 0, tl))

                P.phase = 'moe'
                blocks = [(ex, ni) for ex in (range(NEXP) if 'moe' in phases else []) for ni in range(len(CNT))]
                ew = {}

                def exp_w(ex):
                    if ex not in ew:
                        ew[ex] = (wload(w_eg[l, ex].rearrange("(kc p) n -> p kc n", p=128), (8, 512), "weg"),
                                  wload(w_eu[l, ex].rearrange("(kc p) n -> p kc n", p=128), (8, 512), "weu"),
                                  wload(w_ed[l, ex].rearrange("(kc p) n -> p kc n", p=128), (4, 1024), "wed"))
                    return ew[ex]

                def moe_gu(b):
                    ex, ni = blocks[b]
                    a, n, t0, ntl = CNT[ni]
                    par = b % 2
                    (weg, keg), (weu, keu), (wed, ked) = exp_w(ex)
                    P.add("pe", I("matmul", pb[6][:, 0:n], lhsT=csel[:, ex * 128:(ex + 1) * 128], rhs=combT[:, a:a + n], start=True, stop=True),
                          reads=["csel"] + tk("combT", 0, t0, ntl), writes=[("pb", 6)])
                    P.add("act", I("activation", out=ft[par][:, 0:n], in_=pb[6][:, 0:n], func=AF.Copy),
                          reads=[("pb", 6)], writes=[("ft", par)])
                    for fc in range(4):
                        pg_, pu_ = pb[(fc % 2) * 2], pb[(fc % 2) * 2 + 1]
                        for kc in range(8):
                            P.add("pe", I("matmul", pg_[:, 0:n], lhsT=weg[:, kc, fc * 128:(fc + 1) * 128], rhs=hnT[:, kc, a:a + n], start=(kc == 0), stop=(kc == 7)),
                                  reads=[keg] + tk("hn", kc, t0, ntl), writes=[("pb", (fc % 2) * 2)])
                        for kc in range(8):
                            P.add("pe", I("matmul", pu_[:, 0:n], lhsT=weu[:, kc, fc * 128:(fc + 1) * 128], rhs=hnT[:, kc, a:a + n], start=(kc == 0), stop=(kc == 7)),
                                  reads=[keu] + tk("hn", kc, t0, ntl), writes=[("pb", (fc % 2) * 2 + 1)])
                        fs, fu = ft[2 + fc % 2], ft[4 + fc % 2]
                        P.add("act", I("activation", out=fs[:, 0:n], in_=pg_[:, 0:n], func=AF.Silu),
                              reads=[("pb", (fc % 2) * 2)], writes=[("ft", 2 + fc % 2)])
                        P.add("dve", I("tensor_tensor", out=fu[:, 0:n], in0=pu_[:, 0:n], in1=fs[:, 0:n], op=ALU.mult),
                              reads=[("pb", (fc % 2) * 2 + 1), ("ft", 2 + fc % 2)], writes=[("ft", 4 + fc % 2)])
                        P.add("dve", I("tensor_tensor", out=bt[par * 4 + fc][:, 0:n], in0=fu[:, 0:n], in1=ft[par][:, 0:n], op=ALU.mult),
                              reads=[("ft", 4 + fc % 2), ("ft", par)], writes=[("bt", par * 4 + fc)])

                def moe_dn(b):
                    ex, ni = blocks[b]
                    a, n, t0, ntl = CNT[ni]
                    par = b % 2
                    (weg, keg), (weu, keu), (wed, ked) = exp_w(ex)
                    for m in range(8):
                        bi = 4 + (b * 8 + m) % 3
                        pd = pb[bi]
                        for fc in range(4):
                            P.add("pe", I("matmul", pd[:, 0:n], lhsT=wed[:, fc, m * 128:(m + 1) * 128], rhs=bt[par * 4 + fc][:, 0:n], start=(fc == 0), stop=(fc == 3)),
                                  reads=[ked, ("bt", par * 4 + fc)], writes=[("pb", bi)])
                        P.add("dve", I("tensor_tensor", out=hT[:, m, a:a + n], in0=pd[:, 0:n], in1=hT[:, m, a:a + n], op=ALU.add),
                              reads=[("pb", bi)] + tk("h", m, t0, ntl), writes=tk("h", m, t0, ntl))

                if blocks:
                    moe_gu(0)
                for b in range(len(blocks)):
                    if b + 1 < len(blocks):
                        moe_gu(b + 1)
                    moe_dn(b)

                if debug == ("moe", l) and seq == 0 and gi == 0:
                    P.add("sp", I("dma_start", out=dbg[:, :, 0:T], in_=hT[:, :, 0:T]), reads=tk8("h", 0, gn), group="dbg")

            P.phase = 'final'
            tl = 1 if g0 == 0 else 0
            while tl < gn:
                ntl = min(2, gn - tl)
                a, n = tl * 128, ntl * 128
                norm_ntile(PV_FN, a, n, tl, ntl,
                           lambda c, n=n: ft[2 + c // 2][:, (c % 2) * 256:(c % 2) * 256 + n],
                           lambda c: [("ft", 2 + c // 2)])
                for j in range(ntl):
                    gt = g0 + tl + j
                    for c in range(8):
                        b = c // 4
                        o = (c % 2) * 256 + j * 128
                        P.add("pe", I("transpose", out=pb[b][:, (c % 4) * 128:(c % 4 + 1) * 128], in_=ft[2 + c // 2][:, o:o + 128], identity=ident_f[:]),
                              reads=[("ft", 2 + c // 2), "ident_f"], writes=[("pb", b)])
                    for b in range(2):
                        P.add("act", I("activation", out=ost[:, b * 512:(b + 1) * 512], in_=pb[b][:, 0:512], func=AF.Copy),
                              reads=[("pb", b)], writes=["xin"])
                    P.add("sp", I("dma_start", out=out[seq, (gt - 1) * 128:gt * 128, :], in_=ost[:]),
                          reads=["xin"], group="xin")
                tl += ntl

    P.emit()
    P.close()
    nc._prog_ops = P.ops
    return nc


def host_constants():
    j = np.arange(128)[:, None]
    i = np.arange(128)[None, :]
    m_lo = (i >= j).astype(np.uint8)
    m_up = ((i < j) & (i // 64 == j // 64)).astype(np.uint8)
    cmask = np.stack([m_lo, m_up]).astype(np.uint8)
    cpool = np.zeros((12, 128, 128), np.float32)
    s = np.arange(128)[:, None]
    t = np.arange(128)[None, :]
    for g, w in enumerate((2, 4, 8, 16)):
        dcur = t - s
        cpool[g] = np.where((dcur >= 0) & (dcur < w), 1.0 / w, 0.0) - (dcur == 0)
        dprev = t + 128 - s
        cpool[4 + g] = np.where(dprev < w, 1.0 / w, 0.0)
        tseq = t - 112
        cnt = np.minimum(tseq + 1, w).astype(np.float32)
        cnt = np.where(cnt > 0, cnt, 1.0)
        cpool[8 + g] = np.where((dcur >= 0) & (dcur < w) & (s >= 112), 1.0 / cnt, 0.0) - ((dcur == 0) & (s >= 112))
    csel = np.zeros((32, NEXP, 128), np.float32)
    for e in range(NEXP):
        csel[e, e, :] = 1.0
        csel[16 + e, e, :] = 1.0
    csel = csel.reshape(32, NEXP * 128)
    cident = np.eye(128, dtype=np.float32)
    return cmask, cpool, csel, cident


def host_pvec(norm1_w, norm2_w, pool_scale, b_gate, gla_norm_w, final_norm_w):
    pv = np.zeros((128, PV_COLS), np.float32)
    for l in range(NL):
        o = l * PV_L
        pv[:, o + PV_N1:o + PV_N1 + 8] = np.asarray(norm1_w[l]).reshape(8, 128).T
        pv[:, o + PV_N2:o + PV_N2 + 8] = np.asarray(norm2_w[l]).reshape(8, 128).T
        pv[:, o + PV_PS:o + PV_PS + 8] = np.asarray(pool_scale[l]).reshape(8, 128).T
        pv[:, o + PV_BG:o + PV_BG + 4] = np.asarray(b_gate[l]).reshape(4, 128).T
        pv[:, o + PV_GN:o + PV_GN + 2] = np.asarray(gla_norm_w[l]).reshape(2, 128).T
    pv[:, PV_FN:PV_FN + 8] = np.asarray(final_norm_w).reshape(8, 128).T
    return pv


_NC_CACHE = {}


def make_in_maps(inputs, n_cores=8):
    f = lambda k: np.ascontiguousarray(np.asarray(inputs[k], dtype=np.float32))
    cmask, cpool, csel, cident = host_constants()
    pv = host_pvec(f("norm1_w"), f("norm2_w"), f("pool_scale"), f("b_gate"), f("gla_norm_w"), f("final_norm_w"))
    w_router = np.ascontiguousarray(np.concatenate([f("w_router_group"), f("w_router_expert")], axis=-1))
    shared = {
        "meta": f("meta_tokens"), "w_in": f("w_in"), "w_gate_up": f("w_gate_up"), "w_pool_grp": f("w_pool_grp"),
        "w_br_gla": f("w_br_gla"), "w_br_pool": f("w_br_pool"), "w_out": f("w_out"), "w_router": w_router,
        "w_exp_gate": f("w_exp_gate"), "w_exp_up": f("w_exp_up"), "w_exp_down": f("w_exp_down"),
        "pvec": pv, "cmask": cmask, "cpool": cpool, "csel": csel, "cident": cident,
    }
    x = f("x")
    maps = []
    for c in range(n_cores):
        m = dict(shared)
        m["x"] = np.ascontiguousarray(x[2 * c:2 * c + 2])
        maps.append(m)
    return maps


def kernel(**inputs):
    if "nc" not in _NC_CACHE:
        _NC_CACHE["nc"] = build_program()
    nc = _NC_CACHE["nc"]
    maps = make_in_maps(inputs, 8)
    res = run_bass_kernel_spmd(nc, maps, core_ids=list(range(8)))
    outs = [np.asarray(r["out"]) for r in res.results]
    return np.concatenate(outs, axis=0).astype(np.float32)
```
